# Optimizing a Trainium2 kernel written in Bass

```python
import math
import jax, jax.numpy as jnp
from jax import lax
import numpy as np

D_MODEL = 1024
BATCH = 32
SEQ = 2048
DEPTH = 2

FOX_HEADS = 4
FOX_HEAD_DIM = 64
MOBA_HEADS = 4
MOBA_HEAD_DIM = 64
MOBA_BLOCK = 256
MOBA_TOPK = 3
MOBA_Q_CHUNK = 32
MLA_HEADS = 8
MLA_NOPE_DIM = 64
MLA_ROPE_DIM = 32
MLA_V_DIM = 64
MLA_Q_RANK = 256
MLA_KV_RANK = 128
ROPE_THETA = 10000.0
T5_BUCKETS = 32
T5_MAX_DISTANCE = 128
Q_BLOCK = 128
D_FF = -(-8 * D_MODEL // (3 * 256)) * 256
RMS_EPS = 1e-6

FOX_W = FOX_HEADS * FOX_HEAD_DIM
MOBA_W = MOBA_HEADS * MOBA_HEAD_DIM
MLA_W = MLA_HEADS * MLA_V_DIM
MIX_WIDTH = FOX_W + MOBA_W + MLA_W
IN_SIZES = (FOX_W, FOX_W, FOX_W, FOX_HEADS, MOBA_W, MOBA_W, MOBA_W, MLA_Q_RANK, MLA_KV_RANK, MLA_ROPE_DIM)
IN_WIDTH = 3 * FOX_W + FOX_HEADS + 3 * MOBA_W + MLA_Q_RANK + MLA_KV_RANK + MLA_ROPE_DIM

kernel_name = 'hybrid_fox_moba_mla_block'


def rmsnorm(x, g):
    x32 = x.astype(jnp.float32)
    y = x32 * lax.rsqrt(jnp.mean(x32 * x32, axis=-1, keepdims=True) + RMS_EPS)
    return (y * g.astype(jnp.float32)).astype(x.dtype)


def rope(x, pos):
    half = x.shape[-1] // 2
    inv_freq = 1.0 / (ROPE_THETA ** (jnp.arange(half, dtype=jnp.float32) / half))
    ang = pos.astype(jnp.float32)[:, None] * inv_freq[None, :]
    cos, sin = jnp.cos(ang), jnp.sin(ang)
    x32 = x.astype(jnp.float32)
    x1, x2 = x32[..., :half], x32[..., half:]
    return jnp.concatenate([x1 * cos - x2 * sin, x2 * cos + x1 * sin], axis=-1).astype(x.dtype)


def t5_bucket(dist):
    n = jnp.maximum(dist, 0)
    max_exact = T5_BUCKETS // 2
    nf = jnp.maximum(n, max_exact).astype(jnp.float32)
    large = max_exact + (jnp.log(nf / max_exact) / math.log(T5_MAX_DISTANCE / max_exact)
                         * (T5_BUCKETS - max_exact)).astype(jnp.int32)
    large = jnp.minimum(large, T5_BUCKETS - 1)
    return jnp.where(n < max_exact, n, large)


def dense_causal_attention(q, k, v, log_decay_cum=None):
    B, H, S, dk = q.shape
    dv = v.shape[-1]
    nqb = S // Q_BLOCK
    scale = dk ** -0.5
    kpos = jnp.arange(S)
    q_blocks = q.reshape(B, H, nqb, Q_BLOCK, dk).transpose(2, 0, 1, 3, 4)
    starts = jnp.arange(nqb, dtype=jnp.int32) * Q_BLOCK
    if log_decay_cum is None:
        xs = (q_blocks, starts)
    else:
        xs = (q_blocks, starts, log_decay_cum.reshape(B, H, nqb, Q_BLOCK).transpose(2, 0, 1, 3))

    def step(blk):
        q_i, t0 = blk[0], blk[1]
        s = jnp.einsum('bhqd,bhkd->bhqk', q_i, k).astype(jnp.float32) * scale
        if log_decay_cum is not None:
            s = s + blk[2][..., None] - log_decay_cum[:, :, None, :]
        qpos = t0 + jnp.arange(Q_BLOCK)
        s = jnp.where(kpos[None, :] <= qpos[:, None], s, -jnp.inf)
        p = jax.nn.softmax(s, axis=-1)
        return jnp.einsum('bhqk,bhkd->bhqd', p.astype(v.dtype), v)

    out = lax.map(step, xs)
    return out.transpose(1, 2, 0, 3, 4).reshape(B, H, S, dv)


def moba_attention(q, k, v, t5_table):
    B, H, S, d = q.shape
    nb = -(-S // MOBA_BLOCK)
    s_pad = nb * MOBA_BLOCK
    n_sel = min(MOBA_TOPK, nb)
    scale = d ** -0.5
    pad = ((0, 0), (0, 0), (0, s_pad - S), (0, 0))
    k_blocks = jnp.pad(k, pad).reshape(B, H, nb, MOBA_BLOCK, d)
    v_blocks = jnp.pad(v, pad).reshape(B, H, nb, MOBA_BLOCK, d)
    k_mean = jnp.mean(k_blocks.astype(jnp.float32), axis=3)
    qpos = jnp.arange(S)
    gate = jnp.einsum('bhsd,bhnd->bhsn', q.astype(jnp.float32), k_mean)
    past = jnp.arange(nb)[None, :] < (qpos // MOBA_BLOCK)[:, None]
    gate = jnp.where(past, gate, -jnp.inf)
    _, sel = lax.top_k(gate, n_sel)

    nc = S // MOBA_Q_CHUNK
    q_c = q.reshape(B, H, nc, MOBA_Q_CHUNK, d).transpose(2, 0, 1, 3, 4)
    sel_c = sel.reshape(B, H, nc, MOBA_Q_CHUNK, n_sel).transpose(2, 0, 1, 3, 4)
    starts = jnp.arange(nc, dtype=jnp.int32) * MOBA_Q_CHUNK
    table_t = t5_table.T
    b_ix = jnp.arange(B)[:, None, None, None]
    h_ix = jnp.arange(H)[None, :, None, None]
    offs = jnp.arange(MOBA_BLOCK)

    def step(blk):
        q_i, sel_i, t0 = blk
        own = t0 // MOBA_BLOCK
        qp = t0 + jnp.arange(MOBA_Q_CHUNK)
        k_g = k_blocks[b_ix, h_ix, sel_i]
        v_g = v_blocks[b_ix, h_ix, sel_i]
        kp_sel = sel_i[..., None] * MOBA_BLOCK + offs
        bias_sel = table_t[h_ix[..., None], t5_bucket(qp[:, None, None] - kp_sel)]
        s_sel = jnp.einsum('bhqd,bhqnkd->bhqnk', q_i, k_g).astype(jnp.float32) * scale + bias_sel
        s_sel = jnp.where((sel_i < own)[..., None], s_sel, -jnp.inf)
        k_own = lax.dynamic_index_in_dim(k_blocks, own, axis=2, keepdims=False)
        v_own = lax.dynamic_index_in_dim(v_blocks, own, axis=2, keepdims=False)
        dist_own = qp[:, None] - (own * MOBA_BLOCK + offs)[None, :]
        s_own = (jnp.einsum('bhqd,bhkd->bhqk', q_i, k_own).astype(jnp.float32) * scale
                 + table_t[:, t5_bucket(dist_own)])
        s_own = jnp.where(dist_own >= 0, s_own, -jnp.inf)
        n_flat = n_sel * MOBA_BLOCK
        s = jnp.concatenate([s_sel.reshape(B, H, MOBA_Q_CHUNK, n_flat), s_own], axis=-1)
        p = jax.nn.softmax(s, axis=-1).astype(v.dtype)
        p_sel = p[..., :n_flat].reshape(B, H, MOBA_Q_CHUNK, n_sel, MOBA_BLOCK)
        p_own = p[..., n_flat:]
        return (jnp.einsum('bhqnk,bhqnkd->bhqd', p_sel, v_g)
                + jnp.einsum('bhqk,bhkd->bhqd', p_own, v_own))

    out = lax.map(step, (q_c, sel_c, starts))
    return out.transpose(1, 2, 0, 3, 4).reshape(B, H, S, d)


def hybrid_mixer(h, w_in, b_forget, g_q_lat, w_uq, g_kv_lat, w_ukv, g_group, w_out, t5_table):
    B, S, _ = h.shape
    proj = h @ w_in
    split_points = np.cumsum(IN_SIZES)[:-1].tolist()
    q_f, k_f, v_f, f_logit, q_m, k_m, v_m, c_q, c_kv, k_r = jnp.split(proj, split_points, axis=-1)

    def heads(t, n):
        return t.reshape(B, S, n, -1).transpose(0, 2, 1, 3)

    def flat(o):
        return o.transpose(0, 2, 1, 3).reshape(B, S, -1)

    pos = jnp.arange(S)
    log_f = jax.nn.log_sigmoid((f_logit + b_forget).astype(jnp.float32))
    F = jnp.cumsum(log_f, axis=1).transpose(0, 2, 1)
    o_f = dense_causal_attention(heads(q_f, FOX_HEADS), heads(k_f, FOX_HEADS), heads(v_f, FOX_HEADS), F)
    o_m = moba_attention(heads(q_m, MOBA_HEADS), heads(k_m, MOBA_HEADS), heads(v_m, MOBA_HEADS), t5_table)
    q_lat = heads(rmsnorm(c_q, g_q_lat) @ w_uq, MLA_HEADS)
    q_nope, q_rot = q_lat[..., :MLA_NOPE_DIM], q_lat[..., MLA_NOPE_DIM:]
    kv = heads(rmsnorm(c_kv, g_kv_lat) @ w_ukv, MLA_HEADS)
    k_nope, v_c = kv[..., :MLA_NOPE_DIM], kv[..., MLA_NOPE_DIM:]
    k_rot = rope(k_r[:, None], pos)
    q_c = jnp.concatenate([q_nope, rope(q_rot, pos)], axis=-1)
    k_c = jnp.concatenate([k_nope, jnp.broadcast_to(k_rot, (B, MLA_HEADS, S, MLA_ROPE_DIM))], axis=-1)
    o_c = dense_causal_attention(q_c, k_c, v_c)
    g_a, g_b, g_c = jnp.split(g_group, [FOX_W, FOX_W + MOBA_W])
    o = jnp.concatenate([rmsnorm(flat(o_f), g_a), rmsnorm(flat(o_m), g_b), rmsnorm(flat(o_c), g_c)], axis=-1)
    return o @ w_out


def swiglu(h, w_gate_up, w_down):
    g, u = jnp.split(h @ w_gate_up, 2, axis=-1)
    return (jax.nn.silu(g) * u) @ w_down


def setup_inputs(seed: int = 0) -> dict:
    key = jax.random.key(seed)
    ks = jax.random.split(key, 20)
    f32 = jnp.float32

    def nrm(k, shape, scale):
        return jax.random.normal(k, shape, f32) * scale

    def gain(k, shape):
        return 1.0 + 0.1 * jax.random.normal(k, shape, f32)

    L = DEPTH
    return {
        'x': nrm(ks[0], (BATCH, SEQ, D_MODEL), 1.0),
        'c': nrm(ks[1], (BATCH, D_MODEL), 1.0),
        't5_table': nrm(ks[2], (T5_BUCKETS, MOBA_HEADS), 0.5),
        'w_ada': nrm(ks[3], (L, D_MODEL, 6 * D_MODEL), D_MODEL ** -0.5),
        'b_ada': nrm(ks[4], (L, 6 * D_MODEL), 0.02),
        'g_mix_pre': gain(ks[5], (L, D_MODEL)),
        'g_mix_post': gain(ks[6], (L, D_MODEL)),
        'w_in': nrm(ks[7], (L, D_MODEL, IN_WIDTH), D_MODEL ** -0.5),
        'b_forget': 2.0 + 0.5 * jax.random.normal(ks[8], (L, FOX_HEADS), f32),
        'g_q_lat': gain(ks[9], (L, MLA_Q_RANK)),
        'w_uq': nrm(ks[10], (L, MLA_Q_RANK, MLA_HEADS * (MLA_NOPE_DIM + MLA_ROPE_DIM)), MLA_Q_RANK ** -0.5),
        'g_kv_lat': gain(ks[11], (L, MLA_KV_RANK)),
        'w_ukv': nrm(ks[12], (L, MLA_KV_RANK, MLA_HEADS * (MLA_NOPE_DIM + MLA_V_DIM)), MLA_KV_RANK ** -0.5),
        'g_group': gain(ks[13], (L, MIX_WIDTH)),
        'w_out': nrm(ks[14], (L, MIX_WIDTH, D_MODEL), MIX_WIDTH ** -0.5),
        'g_ffn_pre': gain(ks[15], (L, D_MODEL)),
        'g_ffn_post': gain(ks[16], (L, D_MODEL)),
        'w_gate_up': nrm(ks[17], (L, D_MODEL, 2 * D_FF), D_MODEL ** -0.5),
        'w_down': nrm(ks[18], (L, D_FF, D_MODEL), D_FF ** -0.5),
    }


def reference(x, c, t5_table, w_ada, b_ada, g_mix_pre, g_mix_post, w_in, b_forget, g_q_lat, w_uq,
              g_kv_lat, w_ukv, g_group, w_out, g_ffn_pre, g_ffn_post, w_gate_up, w_down):
    c_act = jax.nn.silu(c)
    for l in range(DEPTH):
        mod = (c_act @ w_ada[l] + b_ada[l])[:, None, :]
        shift_a, scale_a, gate_a, shift_f, scale_f, gate_f = jnp.split(mod, 6, axis=-1)
        h = rmsnorm(x, g_mix_pre[l]) * (1.0 + scale_a) + shift_a
        y = hybrid_mixer(h, w_in[l], b_forget[l], g_q_lat[l], w_uq[l], g_kv_lat[l], w_ukv[l],
                         g_group[l], w_out[l], t5_table)
        x = x + gate_a * rmsnorm(y, g_mix_post[l])
        h = rmsnorm(x, g_ffn_pre[l]) * (1.0 + scale_f) + shift_f
        x = x + gate_f * rmsnorm(swiglu(h, w_gate_up[l], w_down[l]), g_ffn_post[l])
    return x
```

```python
import math
import numpy as np
import concourse.bass as bass
import concourse.mybir as mybir
from concourse.bass_utils import run_bass_kernel_spmd

F32 = mybir.dt.float32
BF16 = mybir.dt.bfloat16
ALU = mybir.AluOpType
AF = mybir.ActivationFunctionType
AX = mybir.AxisListType

S_LEN = 2048
D = 1024
NCORES = 8
NSEQ = 4
DFF = 2816
EPS = 1e-6
NEG = -30000.0


class Sched:
    ENG = ("pe", "act", "dve", "pool", "sp")
    NDMA = 8
    SEM_LIMIT = 30000

    def __init__(self, nc):
        self.nc = nc
        self.eng = {"pe": nc.tensor, "act": nc.scalar, "dve": nc.vector,
                    "pool": nc.gpsimd, "sp": nc.sync}
        self.sem = {}
        self.semkey = {}
        self.cnt = {}
        self.handles = {}
        self.epoch = {e: 0 for e in self.ENG}
        for e in self.ENG:
            self._new_sem(e)
        self.seen = {e: {} for e in self.ENG}
        self.dq = ("sp", "pool")
        self.dsem = {q: [nc.alloc_semaphore(f"dma_{q}{i}") for i in range(self.NDMA)] for q in self.dq}
        self.dcnt = {q: [0] * self.NDMA for q in self.dq}
        self.drr = {q: 0 for q in self.dq}
        for q in self.dq:
            for i in range(self.NDMA):
                self.handles[("d" + q, i)] = self.dsem[q][i]
        self.lastw = {}
        self.readers = {}
        self.ninst = {e: 0 for e in self.ENG}

    def _new_sem(self, e):
        k = (e, self.epoch[e])
        h = self.nc.alloc_semaphore(f"s_{e}_{self.epoch[e]}")
        self.sem[e] = h
        self.semkey[e] = k
        self.cnt[e] = 0
        self.handles[k] = h
        self.epoch[e] += 1

    def _wait(self, e, tok):
        k, v = tok
        if self.seen[e].get(k, 0) >= v:
            return
        if k[0] == e and e in ("pe", "sp"):
            return
        self.eng[e].wait_ge(self.handles[k], v)
        self.ninst[e] += 1
        self.seen[e][k] = v

    def _deps(self, e, reads, writes):
        toks = {}

        def add(t):
            k, v = t
            if toks.get(k, 0) < v:
                toks[k] = v
        for k in reads:
            w = self.lastw.get(k)
            if w:
                add(w)
        for k in writes:
            w = self.lastw.get(k)
            if w:
                add(w)
            for kk, vv in self.readers.get(k, {}).items():
                add((kk, vv))
        for k, v in toks.items():
            self._wait(e, (k, v))

    def _record(self, tok, reads, writes):
        k, v = tok
        for r in reads:
            d = self.readers.setdefault(r, {})
            if d.get(k, 0) < v:
                d[k] = v
        for w in writes:
            self.lastw[w] = tok
            self.readers[w] = {}

    def op(self, e, reads, writes, fn):
        reads = list(reads)
        writes = list(writes)
        if self.cnt[e] >= self.SEM_LIMIT:
            self._new_sem(e)
        self._deps(e, reads, writes)
        ins = fn(self.eng[e])
        self.cnt[e] += 1
        self.ninst[e] += 1
        ins.then_inc(self.sem[e], 1)
        self._record((self.semkey[e], self.cnt[e]), reads, writes)
        return ins

    def dma(self, q, out, in_, reads, writes, **kw):
        reads = list(reads)
        writes = list(writes)
        slot = self.drr[q]
        self.drr[q] = (slot + 1) % self.NDMA
        dk = ("d" + q, slot)
        if self.dcnt[q][slot] > 0:
            self._wait(q, (dk, self.dcnt[q][slot]))
        self._deps(q, reads, writes)
        ins = self.eng[q].dma_start(out=out, in_=in_, **kw)
        self.ninst[q] += 1
        self.dcnt[q][slot] += 16
        ins.then_inc(self.dsem[q][slot], 16)
        self._record((dk, self.dcnt[q][slot]), reads, writes)
        return ins

    def barrier(self):
        toks = []
        for e in self.ENG:
            for ep in range(self.epoch[e]):
                k = (e, ep)
                v = self.cnt[e] if k == self.semkey[e] else self.SEM_LIMIT
                if v > 0:
                    toks.append((k, v))
        for q in self.dq:
            for i in range(self.NDMA):
                if self.dcnt[q][i] > 0:
                    toks.append((("d" + q, i), self.dcnt[q][i]))
        for e in self.ENG:
            for t in toks:
                if t[0][0] == e:
                    continue
                self._wait(e, t)
        self.lastw = {}
        self.readers = {}

    def finish(self):
        self.barrier()


def t5_lo_bounds():
    d = np.arange(0, 400, dtype=np.int32)
    nf = np.maximum(d, 16).astype(np.float32)
    large = 16 + (np.log(nf / np.float32(16)) / np.float32(math.log(128 / 16)) * np.float32(16)).astype(np.int32)
    large = np.minimum(large, 31)
    bucket = np.where(d < 16, d, large)
    lo = []
    for b in range(32):
        idx = np.nonzero(bucket >= b)[0]
        lo.append(int(idx[0]))
    return lo


class _Stop(Exception):
    pass


def build(nseq=NSEQ, nlayers=2, dbg=False, stop=None):
    def ck(tag):
        if stop == tag:
            raise _Stop()
    nc = bass.Bass("TRN2", target_bir_lowering=False)

    def din(name, shape):
        return nc.dram_tensor(name, list(shape), F32, kind="ExternalInput").ap()

    xT_d = din("xT", [nseq, 8, 128, S_LEN])
    cT_d = din("cT", [128, 8 * nseq])
    wada_d = din("w_ada", [2, D, 6 * D])
    bada_d = din("b_adaT", [128, 96])
    gvec_d = din("gvec", [128, 80])
    glat_d = din("glat", [128, 6])
    ggrp_d = din("g_group", [2, D])
    bfor_d = din("bfor", [2, 4])
    t5_d = din("t5T", [1, 128])
    win_d = din("w_in", [2, D, 1956])
    wuq_d = din("w_uq", [2, 256, 768])
    wukv_d = din("w_ukv", [2, 128, 1024])
    wout_d = din("w_out", [2, D, D])
    wgu_d = din("w_gate_up", [2, D, 2 * DFF])
    wdn_d = din("w_down", [2, DFF, D])
    cid_d = din("c_ident", [128, 128])
    cmask_d = din("c_mask", [128, 128])
    cD_d = din("c_D", [128, 256])
    crope_d = din("c_rope", [128, 2, S_LEN])
    outT_d = nc.dram_tensor("outT", [nseq, 8, 128, S_LEN], F32, kind="ExternalOutput").ap()
    dbg_d = {}
    if dbg:
        for nm, shp in (("d_h", [128, 8, S_LEN]), ("d_O", [128, 8, S_LEN]), ("d_xa", [128, 8, S_LEN]),
                        ("d_mod", [128, 96 * nseq])):
            dbg_d[nm] = nc.dram_tensor(nm, shp, F32, kind="ExternalOutput").ap()

    S = Sched(nc)
    A = nc.alloc_sbuf_tensor
    XT = A("XT", [128, 8, S_LEN], F32)
    HBt = A("HB", [128, 16384], BF16)
    HB = HBt[:, :].rearrange("p (a b) -> p a b", a=8)
    BIG = A("BIG", [128, 36864], BF16)

    def bview(off, n):
        return BIG[:, off:off + n]
    QT = [bview(j * 2048, 2048) for j in range(2)]
    KT = [bview(4096 + j * 2048, 2048) for j in range(2)]
    Vt = bview(8192, 4096).rearrange("p (t j d) -> p t j d", t=16, j=2)
    CQN = bview(12288, 4096).rearrange("p (a b) -> p a b", a=2)
    CKVN = bview(16384, 2048)
    KROT = bview(18432, 2048)
    SELT = BIG[32:48, 18432:20480]
    OTt = bview(20480, 16384).rearrange("p (k t) -> p k t", k=8)
    MF = BIG[:, 12288:16384].bitcast(F32)
    FF = BIG[:, 12288:18432].bitcast(F32)
    PRO = BIG[:, 32768:34880].bitcast(F32)
    WO = bview(12288, 8192).rearrange("p (a b) -> p a b", a=8)
    ACTT = bview(0, 22528).rearrange("p (a b) -> p a b", a=22)
    WGU = [bview(22528 + b * 4096, 4096).rearrange("p (k g n) -> p k g n", k=8, g=2) for b in range(2)]
    WD = [bview(30720 + b * 2816, 2816).rearrange("p (k n) -> p k n", k=22) for b in range(2)]
    BIGF = BIG[:, 0:32768].bitcast(F32)
    WADA = [BIGF[:, b * 8192:(b + 1) * 8192].rearrange("p (k n) -> p k n", k=8) for b in range(2)]

    ident_f = A("ident_f", [128, 128], F32)
    ident_b = A("ident_b", [128, 128], BF16)
    ones_b = A("ones_b", [128, 128], BF16)
    mask_b = A("mask_b", [128, 128], BF16)
    mask_f = PRO[:, 0:128]
    Dt = PRO[:, 128:384]
    Esel = A("Esel", [48, 16, 128], BF16)
    BIAS = A("BIAS", [128, 4, 2, 2, 128], BF16)
    tbl = PRO[:, 384:512].rearrange("p (h b) -> p h b", h=4)
    dlt = PRO[:, 512:640].rearrange("p (h b) -> p h b", h=4)
    gvec = A("gvec_s", [128, 5, 2, 8], F32)
    glat = A("glat_s", [128, 6], F32)
    bfor = A("bfor_s", [2, 4], F32)
    badaT = PRO[:, 640:736]
    cact = PRO[:, 736:736 + 8 * nseq].rearrange("p (k b) -> p k b", k=8)
    MODT = A("MODT", [128, 2, 48, nseq], F32)
    vec = A("vec", [128, 6, 8], F32)
    wst = A("wst", [128, 8, 416], BF16)
    wkrR = A("wkrR", [128, 8, 32], BF16)
    wuq = A("wuq_s", [128, 2, 192], BF16)
    wuqR = A("wuqR", [128, 2, 192], BF16)
    wukv = A("wukv_s", [128, 256], BF16)
    rope = A("rope", [128, 2, 512], F32)
    sq = [A(f"sq{i}", [128, 512], BF16) for i in range(2)]
    tmpf = [A(f"tmpf{i}", [128, 512], F32) for i in range(3)]
    lnv = A("lnv", [128, 512], F32)
    rstd = A("rstd", [128, 512], F32)
    PT = [A(f"PT{i}", [128, 512], BF16) for i in range(3)]
    Q32 = MF[:, 0:512]
    ksum = A("ksum", [128, 8], F32)
    Gt = MF[:, 512:768].rearrange("p (a n) -> p a n", a=32)
    cmpt = MF[:, 768:1280].rearrange("p (a n m) -> p a n m", a=8, n=8)
    rank = MF[:, 1280:1536].rearrange("p (a n) -> p a n", a=32)
    selneg = MF[:, 1536:1792].rearrange("p (a n) -> p a n", a=32)
    fe = FF[0:2, 0:512]
    fg = FF[0:2, 512:1024]
    Gc = [FF[0:2, 1024 + i * 512:1536 + i * 512] for i in range(2)]
    fR = FF[0:2, 2048:2560]
    fRall = BIG[0:2, 32768:36864].bitcast(F32)
    fhi = BIG[0:2, 17408:17920]
    fnhi = BIG[0:2, 17920:18432]
    bkT = A("bkT", [128, 16, 2], F32)
    negb = A("negb", [2, 4], F32)

    P = nc.alloc_psum_tensor
    psA = [P(f"psA{i}", [128, 512], F32) for i in range(3)]
    psPV = [P(f"psPV{i}", [128, 4, 128], F32) for i in range(2)]
    psM = [P(f"psM{i}", [128, 512], F32) for i in range(3)]
    rrA = [0]
    rrN = [0]
    psPVf = [t_[:, :, :].rearrange("p a b -> p (a b)") for t_ in psPV]
    rrM = [0]
    rrP = [0]
    rrS = [0]
    rrT = [0]

    def nextA():
        i = rrA[0] % 3
        rrA[0] += 1
        return psA[i], f"psA{i}"

    def nextM():
        i = rrM[0] % 3
        rrM[0] += 1
        return psM[i], f"psM{i}"

    def nextPT():
        i = rrP[0] % 3
        rrP[0] += 1
        return PT[i], f"PT{i}"

    def nextsq():
        i = rrS[0] % 2
        rrS[0] += 1
        return sq[i], f"sq{i}"

    def nexttmp():
        i = rrT[0] % 3
        rrT[0] += 1
        return tmpf[i], f"tmpf{i}"

    op = S.op

    def mm(out, lhsT, rhs, start, stop, r, w):
        return op("pe", r, w, lambda e: e.matmul(out, lhsT=lhsT, rhs=rhs, start=start, stop=stop,
                                                 skip_group_check=True))

    def cs(tc_):
        return slice(tc_ * 512, (tc_ + 1) * 512)

    S.dma("sp", ident_f[:], cid_d, [], ["ident_f"])
    S.dma("pool", ident_b[:], cid_d, [], ["ident_b"])
    S.dma("pool", mask_b[:], cmask_d, [], ["mask_b"])
    S.dma("sp", mask_f[:], cmask_d, [], ["mask_f"])
    S.dma("sp", Dt[:], cD_d, [], ["Dt"])
    S.dma("sp", tbl[:].rearrange("p h b -> p (h b)"), t5_d.partition_broadcast(128), [], ["tbl"])
    S.dma("sp", gvec[:].rearrange("p a l k -> p (a l k)"), gvec_d, [], ["gvec"])
    S.dma("sp", glat[:], glat_d, [], ["glat"])
    S.dma("sp", bfor[:], bfor_d, [], ["bfor"])
    S.dma("sp", badaT[:], bada_d, [], ["badaT"])
    S.dma("sp", PRO[:, 736:736 + 8 * nseq], cT_d, [], ["cact"])
    op("dve", [], ["ones_b"], lambda e: e.memset(ones_b[:], 1.0))
    op("dve", ["ident_b"], ["Esel"], lambda e: e.tensor_copy(
        out=Esel[32:48, :, :], in_=ident_b[32:48, 32:48].unsqueeze(2).to_broadcast([16, 16, 128])))
    op("dve", ["bfor"], ["negb"], lambda e: e.tensor_scalar(
        out=negb[:], in0=bfor[:], scalar1=-1.0, scalar2=None, op0=ALU.mult))
    op("act", ["cact"], ["cact"], lambda e: e.activation(out=cact[:], in_=cact[:], func=AF.Silu))

    lo_b = t5_lo_bounds()
    op("dve", ["tbl"], ["dlt"], lambda e: e.tensor_tensor(out=dlt[:, :, 1:32], in0=tbl[:, :, 1:32],
                                                          in1=tbl[:, :, 0:31], op=ALU.subtract))
    op("dve", ["tbl", "dlt"], ["dlt"], lambda e: e.tensor_tensor(out=dlt[:, :, 0:1], in0=tbl[:, :, 0:1],
                                                                 in1=tbl[:, :, 31:32], op=ALU.subtract))
    op("dve", ["dlt"], ["dlt"], lambda e: e.tensor_scalar(out=dlt[:, :, :], in0=dlt[:, :, :], scalar1=8.0, scalar2=None,
                                                          op0=ALU.mult))
    acc = tmpf[0]
    t2 = tmpf[1]
    for h in range(4):
        for ty in range(2):
            Dv = Dt[:, ty * 128:(ty + 1) * 128]
            op("dve", ["dlt", "Dt"], ["tmpf0"], lambda e, h=h: e.tensor_scalar(
                out=acc[:, 0:128], in0=Dv, scalar1=0.0, scalar2=dlt[:, h, 0:1], op0=ALU.mult, op1=ALU.add))
            for b in range(1, 32):
                if lo_b[b] > 255:
                    continue
                op("dve", ["dlt", "Dt"], ["tmpf1"], lambda e, h=h, b=b, Dv=Dv: e.tensor_scalar(
                    out=t2[:, 0:128], in0=Dv, scalar1=float(lo_b[b]) - 0.5, scalar2=dlt[:, h, b:b + 1],
                    op0=ALU.is_ge, op1=ALU.mult))
                op("dve", ["tmpf1", "tmpf0"], ["tmpf0"], lambda e: e.tensor_tensor(
                    out=acc[:, 0:128], in0=acc[:, 0:128], in1=t2[:, 0:128], op=ALU.add))
            op("dve", ["tmpf0"], ["BIAS"], lambda e, h=h, ty=ty: e.tensor_copy(out=BIAS[:, h, ty, 0, :], in_=acc[:, 0:128]))
            op("dve", ["tmpf0", "BIAS"], ["tmpf1"], lambda e, h=h, ty=ty: e.tensor_tensor(
                out=t2[:, 0:128], in0=acc[:, 0:128], in1=BIAS[:, h, ty, 0, :], op=ALU.subtract))
            op("dve", ["tmpf1"], ["BIAS"], lambda e, h=h, ty=ty: e.tensor_copy(out=BIAS[:, h, ty, 1, :], in_=t2[:, 0:128]))
            if ty == 0:
                op("dve", ["BIAS", "mask_f"], ["BIAS"], lambda e, h=h: e.tensor_tensor(
                    out=BIAS[:, h, 0, 0, :], in0=BIAS[:, h, 0, 0, :], in1=mask_f[:], op=ALU.add))

    for l in range(nlayers):
        for sl in range(6):
            b = (l * 6 + sl) % 2
            wk = f"wada{b}"
            S.dma("sp", WADA[b], wada_d[l].rearrange("(k p) n -> p k n", p=128)[:, :, sl * 1024:(sl + 1) * 1024],
                  [], [wk])
            ps, pk = nextM()
            for nn in range(8):
                for kc in range(8):
                    mm(ps[:, nn * nseq:(nn + 1) * nseq], WADA[b][:, kc, nn * 128:(nn + 1) * 128], cact[:, kc, :],
                       kc == 0, kc == 7, [wk, "cact"], [pk])
            op("dve", [pk, "badaT"], ["MODT"], lambda e, ps=ps, l=l, sl=sl: e.tensor_tensor(
                out=MODT[:, l, sl * 8:(sl + 1) * 8, :],
                in0=ps[:, 0:8 * nseq].rearrange("p (n b) -> p n b", n=8),
                in1=badaT[:, l * 48 + sl * 8: l * 48 + (sl + 1) * 8].unsqueeze(2).to_broadcast([128, 8, nseq]),
                op=ALU.add))
    if dbg:
        S.dma("sp", dbg_d["d_mod"], MODT[:].rearrange("p l n b -> p (l n b)"), ["MODT"], [])

    S.barrier()
    try:
        ck("pro")
    except _Stop:
        S.finish()
        return nc, S

    def rstd_from_ps(ps, pk, nfeat):
        op("act", [pk], ["lnv"], lambda e: e.activation(out=lnv[:], in_=ps[:], func=AF.Ln,
                                                         scale=1.0 / nfeat, bias=EPS))
        op("act", ["lnv"], ["rstd"], lambda e: e.activation(out=rstd[:], in_=lnv[:], func=AF.Exp, scale=-0.5))

    def norm_to_hb(tc_, si, bi):
        xk = f"x.{tc_}"
        ps, pk = nextM()
        for kc in range(8):
            s_, sk = nextsq()
            op("act", [xk], [sk], lambda e, s_=s_, kc=kc: e.activation(out=s_[:], in_=XT[:, kc, cs(tc_)], func=AF.Square))
            mm(ps[:], ones_b[:], s_[:], kc == 0, kc == 7, [sk, "ones_b"], [pk])
        rstd_from_ps(ps, pk, D)
        for kc in range(8):
            t_, tk = nexttmp()
            op("dve", [xk, "rstd"], [tk], lambda e, t_=t_, kc=kc: e.tensor_tensor(
                out=t_[:], in0=XT[:, kc, cs(tc_)], in1=rstd[:], op=ALU.mult))
            op("dve", [tk, "vec"], [f"h.{tc_}"], lambda e, t_=t_, kc=kc: e.tensor_scalar(
                out=HB[:, kc, cs(tc_)], in0=t_[:], scalar1=vec[:, si, kc:kc + 1], scalar2=vec[:, bi, kc:kc + 1],
                op0=ALU.mult, op1=ALU.add))

    def proj_fm(wcols, M, tc_, wkey, rd_extra=()):
        ps, pk = nextA()
        for kc in range(8):
            mm(ps[0:M, :], wst[:, kc, wcols], HB[:, kc, cs(tc_)], kc == 0, kc == 7,
               [wkey, f"h.{tc_}"] + list(rd_extra), [pk])
        return ps, pk

    def fox_cols(st):
        return [(0, 128 * st, 128), (128, 256 + 128 * st, 128), (256, 512 + 128 * st, 128), (384, 768 + 2 * st, 2)]

    def moba_cols(hp):
        return [(0, 772 + 128 * hp, 128), (128, 1028 + 128 * hp, 128), (256, 1284 + 128 * hp, 128)]

    def load_mla_w(l, ms):
        S.dma("pool", wuq[:], wuq_d[l].rearrange("(k p) n -> p k n", p=128)[:, :, 192 * ms:192 * ms + 192], [], ["wuq"])
        S.dma("pool", wukv[:], wukv_d[l][:, 256 * ms:256 * ms + 256], [], ["wukv"])

    def load_wst(l, colspecs):
        wv = win_d[l].rearrange("(k p) n -> p k n", p=128)
        for (dst0, src0, n) in colspecs:
            S.dma("pool", wst[:, :, dst0:dst0 + n], wv[:, :, src0:src0 + n], [], ["wst"])

    def v_proj(l_unused, wsl, src_kind):
        op("dve", [f"V.{c}" for c in range(4)], [f"V.{c}" for c in range(4)],
           lambda e: e.memset(Vt[:, :, :, 64:128], 1.0))
        for t in range(16):
            ps, pk = nextM()
            if src_kind == "h":
                for kc in range(8):
                    mm(ps[:, 0:128], HB[:, kc, t * 128:(t + 1) * 128], wst[:, kc, wsl], kc == 0, kc == 7,
                       ["wst", f"h.{t // 4}"], [pk])
                src = ps[:, 0:128].rearrange("p (j d) -> p j d", j=2)
            else:
                rhs = wukv[:, :].rearrange("p (j d) -> p j d", j=2)[:, :, 64:128]
                mm(ps[:, 0:128].rearrange("p (j d) -> p j d", j=2), CKVN[:, t * 128:(t + 1) * 128], rhs, True, True,
                   ["wukv", f"ckvn.{t // 4}"], [pk])
                src = ps[:, 0:128].rearrange("p (j d) -> p j d", j=2)
            op("act", [pk], [f"V.{t // 4}"], lambda e, t=t, src=src: e.activation(out=Vt[:, t, :, 0:64], in_=src, func=AF.Identity))

    def attention_stage(heads):
        steps = [(hd, c, kt) for hd in heads for c in range(4) for kt in range(4 * c + 4)]

        apool = [(psA[0], "psA0"), (psA[1], "psA1"), (psA[2], "psA2"), (psM[0], "psM0")]
        ppool = [(PT[0], "PT0"), (PT[1], "PT1"), (PT[2], "PT2"), (sq[0], "sq0"), (sq[1], "sq1")]
        rr_ = [0, 0]

        def emit_qk(step):
            (j, kind, hglob, kdim, scale, ocol), c, kt = step
            if kind == "moba":
                qsrc, ksrc = QT[0], KT[0]
                p0, p1 = 64 * j, 64 * j + 64
                qk_ = ["Q0", "K0"]
            else:
                qsrc, ksrc = QT[j], KT[j]
                p0, p1 = 0, kdim
                qk_ = [f"Q{j}", f"K{j}"]
            jq0 = max(4 * c, kt)
            q0 = jq0 * 128
            qend = (4 * c + 4) * 128
            W = qend - q0
            ps, pk = apool[rr_[0] % 4]
            rr_[0] += 1
            rd = [f"{qk_[0]}.{c}", f"{qk_[1]}.{kt // 4}"]
            lk = ksrc[p0:p1, kt * 128:(kt + 1) * 128]
            diag = kt >= 4 * c
            mm(ps[:, 0:W], lk, qsrc[p0:p1, q0:qend], True, False, rd, [pk])
            if kind == "moba":
                n = kt // 2
                mm(ps[:, 0:W], Esel[32:48, 8 * j + n, :], SELT[:, q0:qend], False, False,
                   ["Esel", f"selT.{c}"], [pk])
                if diag:
                    mm(ps[:, 0:128], ident_b[:], BIAS[:, hglob, 0, 0, :], False, False, ["ident_b", "BIAS"], [pk])
                    mm(ps[:, 0:128], ident_b[:], BIAS[:, hglob, 0, 1, :], False, False, ["ident_b", "BIAS"], [pk])
                if kt + 1 <= 4 * c + 3 and kt + 1 >= jq0:
                    o_ = (kt + 1) * 128 - q0
                    mm(ps[:, o_:o_ + 128], ident_b[:], BIAS[:, hglob, 1, 0, :], False, False, ["ident_b", "BIAS"], [pk])
                    mm(ps[:, o_:o_ + 128], ident_b[:], BIAS[:, hglob, 1, 1, :], False, True, ["ident_b", "BIAS"], [pk])
            else:
                if diag:
                    mm(ps[:, 0:128], ident_b[:], mask_b[:], False, True, ["ident_b", "mask_b"], [pk])
            pt, ptk = ppool[rr_[1] % 5]
            rr_[1] += 1
            if kind == "fox":
                op("act", [pk, "bkT"], [ptk], lambda e: e.activation(
                    out=pt[:, 0:W], in_=ps[:, 0:W], func=AF.Exp, scale=scale, bias=bkT[:, kt, j:j + 1]))
            else:
                op("act", [pk], [ptk], lambda e: e.activation(
                    out=pt[:, 0:W], in_=ps[:, 0:W], func=AF.Exp, scale=scale))
            return pt, ptk, jq0, q0

        def emit_pv(step, st_):
            (j, kind, hglob, kdim, scale, ocol), c, kt = step
            pt, ptk, jq0, q0 = st_
            acc_ = psPVf[c % 2]
            ak = f"psPV{c % 2}"
            W = (4 * c + 4) * 128 - q0
            off = q0 - 512 * c
            mm(acc_[:, off:off + W], Vt[:, kt, j, :], pt[:, 0:W], kt == 0, kt == 4 * c + 3,
               [ptk, f"V.{kt // 4}"], [ak])
            if kt == 4 * c + 3:
                i_ = rrN[0] % 2
                rrN[0] += 1
                rb, rbk = ((tmpf[0], "tmpf0"), (tmpf[1], "tmpf1"))[i_]
                op("dve", [ak], [rbk], lambda e: e.reciprocal(out=rb[0:64, :], in_=acc_[64:128, :]))
                pj = 64 * ((ocol % 128) // 64)
                op("dve", [ak, rbk], [f"OT.{c}"], lambda e: e.tensor_tensor(
                    out=OTt[pj:pj + 64, ocol // 128, cs(c)], in0=acc_[0:64, :], in1=rb[0:64, :], op=ALU.mult))

        LOOK = 3
        pend = []
        for si in range(len(steps) + LOOK):
            if si < len(steps):
                pend.append((steps[si], emit_qk(steps[si])))
            if si >= LOOK:
                stp, st_ = pend.pop(0)
                emit_pv(stp, st_)

    def layer(i, l):
        S.barrier()
        md = MODT[:, l, :, i]
        for (vi, mo, gi) in ((0, 8, 0), (3, 32, 2)):
            op("dve", ["MODT", "gvec"], ["vec"], lambda e, vi=vi, mo=mo, gi=gi: e.scalar_tensor_tensor(
                out=vec[:, vi, :], in0=md[:, mo:mo + 8], scalar=1.0, in1=gvec[:, gi, l, :], op0=ALU.add, op1=ALU.mult))
        for (vi, mo) in ((1, 0), (4, 24)):
            op("dve", ["MODT"], ["vec"], lambda e, vi=vi, mo=mo: e.tensor_copy(out=vec[:, vi, :], in_=md[:, mo:mo + 8]))
        for (vi, mo, gi) in ((2, 16, 1), (5, 40, 3)):
            op("dve", ["MODT", "gvec"], ["vec"], lambda e, vi=vi, mo=mo, gi=gi: e.tensor_tensor(
                out=vec[:, vi, :], in0=md[:, mo:mo + 8], in1=gvec[:, gi, l, :], op=ALU.mult))

        load_wst(l, fox_cols(0))
        for tc_ in range(4):
            norm_to_hb(tc_, 0, 1)
        if dbg and i == 0 and l == 0:
            for kc in range(8):
                t_, tk = nexttmp()
                for tc_ in range(4):
                    op("dve", [f"h.{tc_}"], [tk], lambda e, t_=t_, kc=kc, tc_=tc_: e.tensor_copy(out=t_[:], in_=HB[:, kc, cs(tc_)]))
                    S.dma("sp", dbg_d["d_h"][:, kc, cs(tc_)], t_[:], [tk], [])

        ck("norm")
        for st in range(2):
            deferred = []
            for j in range(2):
                op("dve", [f"Q{j}.{c}" for c in range(4)], [f"Q{j}.{c}" for c in range(4)],
                   lambda e, j=j: e.memset(QT[j][64:66, :], 1.0))
                op("dve", [f"K{j}.{c}" for c in range(4)], [f"K{j}.{c}" for c in range(4)],
                   lambda e, j=j: e.memset(KT[j][64:66, :], 1.0))
            for tc_ in range(4):
                ps, pk = proj_fm(slice(0, 128), 128, tc_, "wst")
                for j in range(2):
                    op("dve", [pk], [f"Q{j}.{tc_}"], lambda e, ps=ps, j=j: e.tensor_copy(
                        out=QT[j][0:64, cs(tc_)], in_=ps[64 * j:64 * j + 64, :]))
                ps, pk = proj_fm(slice(128, 256), 128, tc_, "wst")
                for j in range(2):
                    op("dve", [pk], [f"K{j}.{tc_}"], lambda e, ps=ps, j=j: e.tensor_copy(
                        out=KT[j][0:64, cs(tc_)], in_=ps[64 * j:64 * j + 64, :]))
                ps, pk = nextM()
                for kc in range(8):
                    mm(ps[0:2, :], wst[:, kc, 384:386], HB[:, kc, cs(tc_)], kc == 0, kc == 7, ["wst", f"h.{tc_}"], [pk])
                op("act", [pk, "negb"], ["fe"], lambda e, ps=ps: e.activation(
                    out=fe[:], in_=ps[0:2, :], func=AF.Exp, scale=-1.0, bias=negb[:, st * 2 + l: st * 2 + l + 1]))
                op("act", ["fe"], ["fe"], lambda e: e.activation(out=fe[:], in_=fe[:], func=AF.Ln, scale=1.0, bias=1.0))
                op("dve", ["fe"], ["fg"], lambda e: e.tensor_scalar(out=fg[:], in0=fe[:], scalar1=-4.0, scalar2=None, op0=ALU.mult))
                g_ = Gc[tc_ % 2]
                gk = f"Gc{tc_ % 2}"
                gp = Gc[(tc_ + 1) % 2]
                gpk = f"Gc{(tc_ + 1) % 2}"
                if tc_ == 0:
                    op("dve", ["fg"], [gk], lambda e, g_=g_: e.tensor_tensor_scan(
                        out=g_[:], data0=fg[:], data1=fg[:], initial=0.0, op0=ALU.add, op1=ALU.add))
                else:
                    op("dve", ["fg", gpk], [gk], lambda e, g_=g_, gp=gp: e.tensor_tensor_scan(
                        out=g_[:], data0=fg[:], data1=fg[:], initial=gp[:, 511:512], op0=ALU.add, op1=ALU.add))
                op("dve", [gk], ["fhi"], lambda e, g_=g_: e.tensor_copy(out=fhi[:], in_=g_[:]))
                op("dve", ["fhi"], ["fnhi"], lambda e: e.tensor_scalar(out=fnhi[:], in0=fhi[:], scalar1=-1.0, scalar2=None, op0=ALU.mult))
                op("dve", ["fhi", gk], ["fR"], lambda e, g_=g_: e.tensor_tensor(out=fR[:], in0=fhi[:], in1=g_[:], op=ALU.subtract))
                op("dve", ["fR"], ["fR"], lambda e: e.tensor_scalar(out=fR[:], in0=fR[:], scalar1=0.125, scalar2=None, op0=ALU.mult))
                for j in range(2):
                    S.dma("sp", QT[j][64:65, cs(tc_)], fhi[j:j + 1, :], ["fhi"], [f"Q{j}.{tc_}"])
                    S.dma("sp", KT[j][65:66, cs(tc_)], fnhi[j:j + 1, :], ["fnhi"], [f"K{j}.{tc_}"])
                op("dve", ["fR"], [f"fRall.{tc_}"], lambda e, tc_=tc_: e.tensor_copy(out=fRall[:, cs(tc_)], in_=fR[:]))
            ck("foxproj")
            v_proj(l, slice(256, 384), "h")
            load_wst(l, fox_cols(1) if st == 0 else moba_cols(0))
            for tt in range(16):
                ps2, pk2 = nextM()
                op("pe", [f"fRall.{tt // 4}", "ident_f"], [pk2], lambda e, ps2=ps2, tt=tt: e.transpose(
                    out=ps2[:, 0:2], in_=fRall[0:2, tt * 128:(tt + 1) * 128], identity=ident_f[0:2, 0:2]))
                op("dve", [pk2], ["bkT"], lambda e, ps2=ps2, tt=tt: e.tensor_copy(out=bkT[:, tt, :], in_=ps2[:, 0:2]))
            ck("foxv")
            attention_stage([(j, "fox", 2 * st + j, 66, 0.125, (st * 2 + j) * 64) for j in range(2)])
            ck("fox")

        S.barrier()
        ck("fox1")
        for hp in range(2):
            op("dve", [], ["Gt"], lambda e: e.memset(Gt[:], -1e30))
            op("dve", [], ["ksum"], lambda e: e.memset(ksum[:], 0.0))
            ck("m0a")
            for tc_ in range(4):
                ps, pk = proj_fm(slice(128, 256), 128, tc_, "wst")
                op("dve", [pk], [f"K0.{tc_}"], lambda e, ps=ps: e.tensor_copy(out=KT[0][:, cs(tc_)], in_=ps[:, :]))
                ck("m0b")
                op("dve", [pk], ["ksum"], lambda e, ps=ps: e.tensor_reduce(
                    out=ksum[:, 2 * tc_:2 * tc_ + 2], in_=ps[:, :].rearrange("p (b k) -> p b k", b=2), axis=AX.X, op=ALU.add))
                ck("m0c")
                ps, pk = proj_fm(slice(0, 128), 128, tc_, "wst")
                op("dve", [pk], [f"Q0.{tc_}"], lambda e, ps=ps: e.tensor_copy(out=QT[0][:, cs(tc_)], in_=ps[:, :]))
                op("dve", [pk], ["Q32"], lambda e, ps=ps: e.tensor_copy(out=Q32[:], in_=ps[:, :]))
                if tc_ == 0:
                    ck("m1")
                psg, pgk = nextM()
                for t in range(4):
                    for j in range(2):
                        mm(psg[:, (t * 2 + j) * 8:(t * 2 + j) * 8 + 8], Q32[64 * j:64 * j + 64, t * 128:(t + 1) * 128],
                           ksum[64 * j:64 * j + 64, :], True, True, ["Q32", "ksum"], [pgk])
                for half in range(2):
                    own = 2 * tc_ + half
                    if own == 0:
                        continue
                    op("dve", [pgk], ["Gt"], lambda e, psg=psg, half=half, own=own: e.tensor_copy(
                        out=Gt[:, (4 * tc_ + 2 * half) * 2:(4 * tc_ + 2 * half) * 2 + 4, 0:own],
                        in_=psg[:, half * 32:half * 32 + 32].rearrange("p (a n) -> p a n", a=4)[:, :, 0:own]))
            ck("m2")
            for q4 in range(4):
                gv = Gt[:, q4 * 8:(q4 + 1) * 8, :]
                op("dve", ["Gt"], ["cmpt"], lambda e, gv=gv: e.tensor_tensor(
                    out=cmpt[:], in0=gv.unsqueeze(2).to_broadcast([128, 8, 8, 8]),
                    in1=gv.unsqueeze(3).to_broadcast([128, 8, 8, 8]), op=ALU.is_gt))
                op("dve", ["cmpt"], ["rank"], lambda e, q4=q4: e.tensor_reduce(
                    out=rank[:, q4 * 8:(q4 + 1) * 8, :], in_=cmpt[:], axis=AX.X, op=ALU.add))
            op("dve", ["rank"], ["selneg"], lambda e: e.tensor_scalar(
                out=selneg[:], in0=rank[:], scalar1=3.0, scalar2=-NEG, op0=ALU.is_lt, op1=ALU.mult))
            op("dve", ["selneg"], ["selneg"], lambda e: e.tensor_scalar(
                out=selneg[:], in0=selneg[:], scalar1=NEG, scalar2=None, op0=ALU.add))
            for own in range(8):
                op("dve", ["selneg"], ["selneg"], lambda e, own=own: e.memset(selneg[:, own * 4:own * 4 + 4, own:own + 1], 0.0))
            ck("m3")
            v_proj(l, slice(256, 384), "h")
            load_wst(l, moba_cols(1) if hp == 0 else [(0, 1540, 416)])
            for t in range(16):
                ps2, pk2 = nextM()
                op("pe", ["selneg", "ident_f"], [pk2], lambda e, ps2=ps2, t=t: e.transpose(
                    out=ps2[0:16, 0:128], in_=selneg[:, 2 * t:2 * t + 2, :].rearrange("p a n -> p (a n)"), identity=ident_f[:]))
                op("dve", [pk2], [f"selT.{t // 4}"], lambda e, ps2=ps2, t=t: e.tensor_copy(
                    out=SELT[:, t * 128:(t + 1) * 128], in_=ps2[0:16, 0:128]))
            ck("mobaproj")
            attention_stage([(j, "moba", 2 * hp + j, 64, 0.125, 256 + (hp * 2 + j) * 64) for j in range(2)])
            ck("moba")

        S.barrier()
        ck("moba1")
        load_mla_w(l, 0)
        op("dve", ["wst"], ["wkrR"], lambda e: e.tensor_scalar(out=wkrR[:, :, 0:16], in0=wst[:, :, 400:416], scalar1=-1.0,
                                                               scalar2=None, op0=ALU.mult))
        op("dve", ["wst", "wkrR"], ["wkrR"], lambda e: e.tensor_copy(out=wkrR[:, :, 16:32], in_=wst[:, :, 384:400]))
        ck("p1")
        for tc_ in range(4):
            S.dma("sp", rope[:], crope_d[:, :, cs(tc_)], [], ["rope"])
            psn, pnk = nextM()
            for m in range(2):
                ps, pk = proj_fm(slice(128 * m, 128 * m + 128), 128, tc_, "wst")
                op("dve", [pk], [f"tmpf{m}"], lambda e, ps=ps, m=m: e.tensor_copy(out=tmpf[m][:], in_=ps[:, :]))
                s_, sk = nextsq()
                op("act", [f"tmpf{m}"], [sk], lambda e, m=m, s_=s_: e.activation(out=s_[:], in_=tmpf[m][:], func=AF.Square))
                mm(psn[:], ones_b[:], s_[:], m == 0, m == 1, [sk, "ones_b"], [pnk])
            rstd_from_ps(psn, pnk, 256)
            for m in range(2):
                op("dve", [f"tmpf{m}", "rstd", "glat"], [f"cqn.{tc_}"], lambda e, m=m: e.scalar_tensor_tensor(
                    out=CQN[:, m, cs(tc_)], in0=tmpf[m][:], scalar=glat[:, l * 2 + m:l * 2 + m + 1], in1=rstd[:],
                    op0=ALU.mult, op1=ALU.mult))
            ck("p2")
            psn, pnk = nextM()
            ps, pk = proj_fm(slice(256, 384), 128, tc_, "wst")
            op("dve", [pk], ["tmpf2"], lambda e, ps=ps: e.tensor_copy(out=tmpf[2][:], in_=ps[:, :]))
            s_, sk = nextsq()
            op("act", ["tmpf2"], [sk], lambda e, s_=s_: e.activation(out=s_[:], in_=tmpf[2][:], func=AF.Square))
            mm(psn[:], ones_b[:], s_[:], True, True, [sk, "ones_b"], [pnk])
            rstd_from_ps(psn, pnk, 128)
            op("dve", ["tmpf2", "rstd", "glat"], [f"ckvn.{tc_}"], lambda e: e.scalar_tensor_tensor(
                out=CKVN[:, cs(tc_)], in0=tmpf[2][:], scalar=glat[:, 4 + l:5 + l], in1=rstd[:],
                op0=ALU.mult, op1=ALU.mult))
            ck("p3")
            psa, pak = proj_fm(slice(384, 416), 32, tc_, "wst")
            psb, pbk = nextA()
            for kc in range(8):
                mm(psb[0:32, :], wkrR[:, kc, :], HB[:, kc, cs(tc_)], kc == 0, kc == 7, ["wkrR", f"h.{tc_}"], [pbk])
            t1, t1k = nexttmp()
            t2_, t2k = nexttmp()
            op("dve", [pak, "rope"], [t1k], lambda e, psa=psa, t1=t1: e.tensor_tensor(
                out=t1[0:32, :], in0=psa[0:32, :], in1=rope[0:32, 0, :], op=ALU.mult))
            op("dve", [pbk, "rope"], [t2k], lambda e, psb=psb, t2_=t2_: e.tensor_tensor(
                out=t2_[0:32, :], in0=psb[0:32, :], in1=rope[0:32, 1, :], op=ALU.mult))
            op("dve", [t1k, t2k], [f"krot.{tc_}"], lambda e, t1=t1, t2_=t2_: e.tensor_tensor(
                out=KROT[0:32, cs(tc_)], in0=t1[0:32, :], in1=t2_[0:32, :], op=ALU.add))

        ck("mlapre")
        sc_mla = 96 ** -0.5
        for ms in range(4):
            op("dve", [], ["wuqR"], lambda e: e.memset(wuqR[:], 0.0))
            wv = wuq[:].rearrange("p k (j d) -> p k j d", j=2)
            wr = wuqR[:].rearrange("p k (j d) -> p k j d", j=2)
            for k2 in range(2):
                op("dve", ["wuq", "wuqR"], ["wuqR"], lambda e, k2=k2: e.tensor_scalar(
                    out=wr[:, k2, :, 64:80], in0=wv[:, k2, :, 80:96], scalar1=-1.0, scalar2=None, op0=ALU.mult))
                op("dve", ["wuq", "wuqR"], ["wuqR"], lambda e, k2=k2: e.tensor_copy(out=wr[:, k2, :, 80:96], in_=wv[:, k2, :, 64:80]))
            for j in range(2):
                S.dma("sp", KT[j][64:96, :], KROT[0:32, :], [f"krot.{c}" for c in range(4)], [f"K{j}.{c}" for c in range(4)])
            for tc_ in range(4):
                S.dma("sp", rope[:], crope_d[:, :, cs(tc_)], [], ["rope"])
                for j in range(2):
                    psa, pak = nextA()
                    psb, pbk = nextA()
                    for k2 in range(2):
                        mm(psa[0:96, :], wuq[:, k2, 96 * j:96 * j + 96], CQN[:, k2, cs(tc_)], k2 == 0, k2 == 1,
                           ["wuq", f"cqn.{tc_}"], [pak])
                    for k2 in range(2):
                        mm(psb[0:96, :], wuqR[:, k2, 96 * j:96 * j + 96], CQN[:, k2, cs(tc_)], k2 == 0, k2 == 1,
                           ["wuqR", f"cqn.{tc_}"], [pbk])
                    op("dve", [pak], [f"Q{j}.{tc_}"], lambda e, psa=psa, j=j: e.tensor_copy(out=QT[j][0:64, cs(tc_)], in_=psa[0:64, :]))
                    t1, t1k = nexttmp()
                    t2_, t2k = nexttmp()
                    op("dve", [pak, "rope"], [t1k], lambda e, psa=psa, t1=t1: e.tensor_tensor(
                        out=t1[64:96, :], in0=psa[64:96, :], in1=rope[64:96, 0, :], op=ALU.mult))
                    op("dve", [pbk, "rope"], [t2k], lambda e, psb=psb, t2_=t2_: e.tensor_tensor(
                        out=t2_[64:96, :], in0=psb[64:96, :], in1=rope[64:96, 1, :], op=ALU.mult))
                    op("dve", [t1k, t2k], [f"Q{j}.{tc_}"], lambda e, t1=t1, t2_=t2_, j=j: e.tensor_tensor(
                        out=QT[j][64:96, cs(tc_)], in0=t1[64:96, :], in1=t2_[64:96, :], op=ALU.add))
                    psk, pkk = nextA()
                    mm(psk[0:64, :], wukv[:, 128 * j:128 * j + 64], CKVN[:, cs(tc_)], True, True, ["wukv", f"ckvn.{tc_}"], [pkk])
                    op("act", [pkk], [f"K{j}.{tc_}"], lambda e, psk=psk, j=j: e.activation(out=KT[j][0:64, cs(tc_)], in_=psk[0:64, :], func=AF.Identity))
            v_proj(l, None, "kv")
            if ms < 3:
                load_mla_w(l, ms + 1)
            else:
                S.dma("pool", WO, wout_d[l].rearrange("(k p) n -> p k n", p=128), [],
                      ["WO"] + [f"cqn.{c}" for c in range(4)] + [f"ckvn.{c}" for c in range(4)] + [f"krot.{c}" for c in range(4)])
            attention_stage([(j, "mla", 0, 96, sc_mla, 512 + (ms * 2 + j) * 64) for j in range(2)])

        if dbg and i == 0 and l == 0:
            for kc in range(8):
                for tc_ in range(4):
                    t_, tk = nexttmp()
                    op("dve", [f"OT.{tc_}"], [tk], lambda e, t_=t_, kc=kc, tc_=tc_: e.tensor_copy(out=t_[:], in_=OTt[:, kc, cs(tc_)]))
                    S.dma("sp", dbg_d["d_O"][:, kc, cs(tc_)], t_[:], [tk], [])
        S.barrier()
        OTCs = [HB[:, :, 1024:1536], bview(0, 4096).rearrange("p (a b) -> p a b", a=8)]
        rstdG = BIG[:, 4096:5120].bitcast(F32)
        lnvG = BIG[:, 5120:6144].bitcast(F32)
        groups = ((0, 2, 256), (2, 4, 256), (4, 8, 512))
        def gnorm(tc_):
            OTC = OTCs[tc_ % 2]
            otk = f"otc{tc_ % 2}"
            for gi_, (k0, k1, nf) in enumerate(groups):
                psg, pgk = (psM[0], "psM0") if (gi_ + tc_) % 2 == 0 else (psM[1], "psM1")
                for kc in range(k0, k1):
                    s_, sk = nextsq()
                    op("act", [f"OT.{tc_}"], [sk], lambda e, s_=s_, kc=kc: e.activation(
                        out=s_[:], in_=OTt[:, kc, cs(tc_)], func=AF.Square))
                    mm(psg[:], ones_b[:], s_[:], kc == k0, kc == k1 - 1, [sk, "ones_b"], [pgk])
                op("act", [pgk], ["lnvG"], lambda e, psg=psg, nf=nf: e.activation(
                    out=lnvG, in_=psg[:], func=AF.Ln, scale=1.0 / nf, bias=EPS))
                op("act", ["lnvG"], ["rstdG"], lambda e: e.activation(out=rstdG, in_=lnvG, func=AF.Exp, scale=-0.5))
                for kc in range(k0, k1):
                    op("dve", [f"OT.{tc_}", "rstdG", "gvec"], [otk], lambda e, kc=kc, OTC=OTC: e.scalar_tensor_tensor(
                        out=OTC[:, kc, :], in0=OTt[:, kc, cs(tc_)], scalar=gvec[:, 4, l, kc:kc + 1], in1=rstdG,
                        op0=ALU.mult, op1=ALU.mult))
        gnorm(0)
        for tc_ in range(4):
            OTC = OTCs[tc_ % 2]
            otk = f"otc{tc_ % 2}"
            resid_update(tc_, lambda n, OTC=OTC, otk=otk: (WO[:, :, n * 128:(n + 1) * 128], 8, lambda kc: OTC[:, kc, :], ["WO", otk]), 2,
                         lambda n: HB[:, n, 0:1024].bitcast(F32), ["y32a"],
                         mid=(lambda tc_=tc_: gnorm(tc_ + 1)) if tc_ < 3 else None)
        if dbg and i == 0 and l == 0:
            for kc in range(8):
                S.dma("sp", dbg_d["d_xa"][:, kc, :], XT[:, kc, :], [f"x.{c}" for c in range(4)], [])

        ck("outproj")
        S.barrier()
        fpool = [(psA[0], "psA0"), (psA[1], "psA1"), (psA[2], "psA2"), (psM[0], "psM0")]
        frr = [0]
        for hf in range(2):
            for tc_ in (2 * hf, 2 * hf + 1):
                norm_to_hb(tc_, 3, 4)
            for fb in range(11):
                b = fb % 2
                wk = f"wgu{b}"
                wv = wgu_d[l].rearrange("(k p) n -> p k n", p=128)
                S.dma("pool", WGU[b][:, :, 0, :], wv[:, :, fb * 256:(fb + 1) * 256], [], [wk])
                S.dma("pool", WGU[b][:, :, 1, :], wv[:, :, DFF + fb * 256:DFF + (fb + 1) * 256], [], [wk])
                for m in range(2):
                    for hh in range(2):
                        tc_ = 2 * hf + hh
                        psg, pgk = fpool[frr[0] % 4]
                        psu, puk = fpool[(frr[0] + 1) % 4]
                        frr[0] += 2
                        for kc in range(8):
                            mm(psg[:], WGU[b][:, kc, 0, m * 128:(m + 1) * 128], HB[:, kc, cs(tc_)], kc == 0, kc == 7,
                               [wk, f"h.{tc_}"], [pgk])
                        for kc in range(8):
                            mm(psu[:], WGU[b][:, kc, 1, m * 128:(m + 1) * 128], HB[:, kc, cs(tc_)], kc == 0, kc == 7,
                               [wk, f"h.{tc_}"], [puk])
                        pt, ptk = nextPT()
                        op("act", [pgk], [ptk], lambda e, pt=pt, psg=psg: e.activation(out=pt[:], in_=psg[:], func=AF.Silu))
                        op("dve", [ptk, puk], [f"act.{hh}"], lambda e, pt=pt, psu=psu, fb=fb, m=m, hh=hh: e.tensor_tensor(
                            out=ACTT[:, fb * 2 + m, hh * 512:(hh + 1) * 512], in0=pt[:], in1=psu[:], op=ALU.mult))
            resid_ffn2(l, hf)

    def resid_epilogue(tc_, y32f, ykf, psn, pnk, gi):
        rstd_from_ps(psn, pnk, D)
        for n in range(8):
            t_, tk = nexttmp()
            op("dve", ykf(n) + ["rstd"], [tk], lambda e, t_=t_, n=n: e.tensor_tensor(
                out=t_[:], in0=y32f(n), in1=rstd[:], op=ALU.mult))
            op("dve", [tk, "vec", f"x.{tc_}"], [f"x.{tc_}"], lambda e, t_=t_, n=n: e.scalar_tensor_tensor(
                out=XT[:, n, cs(tc_)], in0=t_[:], scalar=vec[:, gi, n:n + 1], in1=XT[:, n, cs(tc_)],
                op0=ALU.mult, op1=ALU.add))

    def resid_update(tc_, wfn, gi, y32f, ykeys, mid=None):
        psn = psM[2]
        pnk = "psM2"
        pend = []

        def flush():
            while pend:
                s0, sk0, n0 = pend.pop(0)
                mm(psn[:], ones_b[:], s0[:], n0 == 0, n0 == 7, [sk0, "ones_b"], [pnk])
        for n in range(8):
            lhs_all, nk, rhsf, rkeys = wfn(n)
            ps, pk = nextA()
            for kc in range(nk):
                mm(ps[:], lhs_all[:, kc, :], rhsf(kc), kc == 0, kc == nk - 1, rkeys, [pk])
            flush()
            op("dve", [pk], [f"y32a.{n}"], lambda e, ps=ps, n=n: e.tensor_copy(out=y32f(n), in_=ps[:]))
            s_, sk = nextsq()
            op("act", [f"y32a.{n}"], [sk], lambda e, n=n, s_=s_: e.activation(out=s_[:], in_=y32f(n), func=AF.Square))
            pend.append((s_, sk, n))
            if n == 2 and mid is not None:
                flush()
                mid()
        flush()
        resid_epilogue(tc_, y32f, lambda n: [f"y32a.{n}"], psn, pnk, gi)

    def resid_ffn2(l, hf):
        psn = [psM[1], psM[2]]
        pnk = ["psM1", "psM2"]
        regs = [hf, 1 - hf]
        coarse = [[f"h.{2 * r}", f"h.{2 * r + 1}"] for r in regs]

        def y32f(hh, n):
            return HB[:, n, regs[hh] * 1024:(regs[hh] + 1) * 1024].bitcast(F32)
        wv = wdn_d[l].rearrange("(k p) n -> p k n", p=128)
        pend = []

        def flush():
            while pend:
                s0, sk0, n0, h0 = pend.pop(0)
                mm(psn[h0][:], ones_b[:], s0[:], n0 == 0, n0 == 7, [sk0, "ones_b"], [pnk[h0]])
        for n in range(8):
            b = n % 2
            wk = f"wd{b}"
            S.dma("pool", WD[b], wv[:, :, n * 128:(n + 1) * 128], [], [wk])
            for hh in range(2):
                ps, pk = nextA()
                for k in range(22):
                    mm(ps[:], WD[b][:, k, :], ACTT[:, k, hh * 512:(hh + 1) * 512], k == 0, k == 21, [wk, f"act.{hh}"], [pk])
                flush()
                yk = [f"y.{hh}.{n}"] + (coarse[hh] if n == 0 else [])
                op("dve", [pk], yk, lambda e, ps=ps, n=n, hh=hh: e.tensor_copy(out=y32f(hh, n), in_=ps[:]))
                s_, sk = nextsq()
                op("act", [f"y.{hh}.{n}"], [sk], lambda e, n=n, hh=hh, s_=s_: e.activation(
                    out=s_[:], in_=y32f(hh, n), func=AF.Square))
                pend.append((s_, sk, n, hh))
        flush()
        for hh in range(2):
            resid_epilogue(2 * hf + hh, lambda n, hh=hh: y32f(hh, n), lambda n, hh=hh: [f"y.{hh}.{n}"] + coarse[hh],
                           psn[hh], pnk[hh], 5)

    psTb = psPV[1][:, :, :].rearrange("p a b -> p (a b)").bitcast(BF16).rearrange("p (k n) -> p k n", k=8)

    try:
        for i in range(nseq):
            for kc in range(8):
                S.dma("sp", XT[:, kc, :], xT_d[i, kc], [], [f"x.{c}" for c in range(4)])
            for l in range(nlayers):
                layer(i, l)
            for kc in range(8):
                S.dma("sp", outT_d[i, kc], XT[:, kc, :], [f"x.{c}" for c in range(4)], [])
    except _Stop:
        pass
    S.finish()
    return nc, S


def host_consts():
    ident = np.eye(128, dtype=np.float32)
    k = np.arange(128)[:, None]
    q = np.arange(128)[None, :]
    mask = np.where(k > q, NEG, 0.0).astype(np.float32)
    Dm = np.concatenate([(q - k), (q - k + 128)], axis=1).astype(np.float32)
    half = 16
    inv_freq = (1.0 / (10000.0 ** (np.arange(half, dtype=np.float32) / half))).astype(np.float32)
    ang = np.arange(S_LEN, dtype=np.float32)[None, :] * inv_freq[:, None]
    cos = np.cos(ang).astype(np.float32)
    sin = np.sin(ang).astype(np.float32)
    rope = np.zeros((128, 2, S_LEN), np.float32)
    for base in (0, 64):
        rope[base:base + 16, 0] = cos
        rope[base + 16:base + 32, 0] = cos
        rope[base:base + 16, 1] = sin
        rope[base + 16:base + 32, 1] = sin
    return ident, mask, Dm, rope


def make_in_maps(inputs, nseq, cores):
    f = lambda a: np.ascontiguousarray(np.asarray(a, dtype=np.float32))
    x = f(inputs["x"])
    c = f(inputs["c"])
    ident, mask, Dm, rope = host_consts()
    b_ada = f(inputs["b_ada"])
    badaT = np.ascontiguousarray(b_ada.reshape(2, 48, 128).transpose(2, 0, 1).reshape(128, 96))
    gv = np.stack([f(inputs[k]) for k in ("g_mix_pre", "g_mix_post", "g_ffn_pre", "g_ffn_post", "g_group")], 0)
    gvec = np.ascontiguousarray(gv.reshape(5, 2, 8, 128).transpose(3, 0, 1, 2).reshape(128, 80))
    gq = f(inputs["g_q_lat"]).reshape(2, 2, 128).transpose(2, 0, 1).reshape(128, 4)
    gkv = f(inputs["g_kv_lat"]).reshape(2, 128).T
    glat = np.ascontiguousarray(np.concatenate([gq, gkv], axis=1))
    bfo = f(inputs["b_forget"])
    bfor = np.zeros((2, 4), np.float32)
    for j in range(2):
        for st in range(2):
            for l in range(2):
                bfor[j, st * 2 + l] = bfo[l, 2 * st + j]
    t5T = np.ascontiguousarray(f(inputs["t5_table"]).T.reshape(1, 128))
    shared = {
        "w_ada": f(inputs["w_ada"]), "b_adaT": badaT, "gvec": gvec, "glat": glat, "g_group": f(inputs["g_group"]),
        "bfor": bfor, "t5T": t5T, "w_in": f(inputs["w_in"]), "w_uq": f(inputs["w_uq"]), "w_ukv": f(inputs["w_ukv"]),
        "w_out": f(inputs["w_out"]), "w_gate_up": f(inputs["w_gate_up"]), "w_down": f(inputs["w_down"]),
        "c_ident": ident, "c_mask": mask, "c_D": Dm, "c_rope": rope,
    }
    maps = []
    for ci in cores:
        xs = x[ci * nseq:(ci + 1) * nseq]
        xT = np.ascontiguousarray(xs.transpose(0, 2, 1).reshape(nseq, 8, 128, S_LEN))
        cs_ = c[ci * nseq:(ci + 1) * nseq]
        cT = np.ascontiguousarray(cs_.T.reshape(8, 128, nseq).transpose(1, 0, 2).reshape(128, 8 * nseq))
        m = dict(shared)
        m["xT"] = xT
        m["cT"] = cT
        maps.append(m)
    return maps


_CACHE = {}


def kernel(**inputs):
    if "nc" not in _CACHE:
        _CACHE["nc"] = build(NSEQ, 2, False)[0]
    nc = _CACHE["nc"]
    maps = make_in_maps(inputs, NSEQ, list(range(NCORES)))
    res = run_bass_kernel_spmd(nc, maps, core_ids=list(range(NCORES)))
    outs = []
    for r in res.results:
        oT = np.asarray(r["outT"]).reshape(NSEQ, D, S_LEN)
        outs.append(oT.transpose(0, 2, 1))
    return np.ascontiguousarray(np.concatenate(outs, axis=0).astype(np.float32))
```

```python
import math
import numpy as np
import concourse.bass as bass
import concourse.mybir as mybir
from concourse.bass_utils import run_bass_kernel_spmd

F32 = mybir.dt.float32
BF16 = mybir.dt.bfloat16
ALU = mybir.AluOpType
AF = mybir.ActivationFunctionType
AX = mybir.AxisListType

S_LEN = 2048
D = 1024
NCORES = 8
NSEQ = 4
DFF = 2816
EPS = 1e-6
NEG = -30000.0


class Sched:
    ENG = ("pe", "act", "dve", "pool", "sp")
    NDMA = 8
    SEM_LIMIT = 30000

    def __init__(self, nc):
        self.nc = nc
        self.eng = {"pe": nc.tensor, "act": nc.scalar, "dve": nc.vector,
                    "pool": nc.gpsimd, "sp": nc.sync}
        self.sem = {}
        self.semkey = {}
        self.cnt = {}
        self.handles = {}
        self.epoch = {e: 0 for e in self.ENG}
        for e in self.ENG:
            self._new_sem(e)
        self.seen = {e: {} for e in self.ENG}
        self.dq = ("sp", "pool")
        self.dsem = {q: [nc.alloc_semaphore(f"dma_{q}{i}") for i in range(self.NDMA)] for q in self.dq}
        self.dcnt = {q: [0] * self.NDMA for q in self.dq}
        self.drr = {q: 0 for q in self.dq}
        for q in self.dq:
            for i in range(self.NDMA):
                self.handles[("d" + q, i)] = self.dsem[q][i]
        self.lastw = {}
        self.readers = {}
        self.ninst = {e: 0 for e in self.ENG}

    def _new_sem(self, e):
        k = (e, self.epoch[e])
        h = self.nc.alloc_semaphore(f"s_{e}_{self.epoch[e]}")
        self.sem[e] = h
        self.semkey[e] = k
        self.cnt[e] = 0
        self.handles[k] = h
        self.epoch[e] += 1

    def _wait(self, e, tok):
        k, v = tok
        if self.seen[e].get(k, 0) >= v:
            return
        if k[0] == e and e in ("pe", "sp"):
            return
        self.eng[e].wait_ge(self.handles[k], v)
        self.ninst[e] += 1
        self.seen[e][k] = v

    def _deps(self, e, reads, writes):
        toks = {}

        def add(t):
            k, v = t
            if toks.get(k, 0) < v:
                toks[k] = v
        for k in reads:
            w = self.lastw.get(k)
            if w:
                add(w)
        for k in writes:
            w = self.lastw.get(k)
            if w:
                add(w)
            for kk, vv in self.readers.get(k, {}).items():
                add((kk, vv))
        for k, v in toks.items():
            self._wait(e, (k, v))

    def _record(self, tok, reads, writes):
        k, v = tok
        for r in reads:
            d = self.readers.setdefault(r, {})
            if d.get(k, 0) < v:
                d[k] = v
        for w in writes:
            self.lastw[w] = tok
            self.readers[w] = {}

    def op(self, e, reads, writes, fn):
        reads = list(reads)
        writes = list(writes)
        if self.cnt[e] >= self.SEM_LIMIT:
            self._new_sem(e)
        self._deps(e, reads, writes)
        ins = fn(self.eng[e])
        self.cnt[e] += 1
        self.ninst[e] += 1
        ins.then_inc(self.sem[e], 1)
        self._record((self.semkey[e], self.cnt[e]), reads, writes)
        return ins

    def dma(self, q, out, in_, reads, writes, **kw):
        reads = list(reads)
        writes = list(writes)
        slot = self.drr[q]
        self.drr[q] = (slot + 1) % self.NDMA
        dk = ("d" + q, slot)
        if self.dcnt[q][slot] > 0:
            self._wait(q, (dk, self.dcnt[q][slot]))
        self._deps(q, reads, writes)
        ins = self.eng[q].dma_start(out=out, in_=in_, **kw)
        self.ninst[q] += 1
        self.dcnt[q][slot] += 16
        ins.then_inc(self.dsem[q][slot], 16)
        self._record((dk, self.dcnt[q][slot]), reads, writes)
        return ins

    def barrier(self):
        toks = []
        for e in self.ENG:
            for ep in range(self.epoch[e]):
                k = (e, ep)
                v = self.cnt[e] if k == self.semkey[e] else self.SEM_LIMIT
                if v > 0:
                    toks.append((k, v))
        for q in self.dq:
            for i in range(self.NDMA):
                if self.dcnt[q][i] > 0:
                    toks.append((("d" + q, i), self.dcnt[q][i]))
        for e in self.ENG:
            for t in toks:
                if t[0][0] == e:
                    continue
                self._wait(e, t)
        self.lastw = {}
        self.readers = {}

    def finish(self):
        self.barrier()


def t5_lo_bounds():
    d = np.arange(0, 400, dtype=np.int32)
    nf = np.maximum(d, 16).astype(np.float32)
    large = 16 + (np.log(nf / np.float32(16)) / np.float32(math.log(128 / 16)) * np.float32(16)).astype(np.int32)
    large = np.minimum(large, 31)
    bucket = np.where(d < 16, d, large)
    lo = []
    for b in range(32):
        idx = np.nonzero(bucket >= b)[0]
        lo.append(int(idx[0]))
    return lo


class _Stop(Exception):
    pass


def build(nseq=NSEQ, nlayers=2, dbg=False, stop=None):
    def ck(tag):
        if stop == tag:
            raise _Stop()
    nc = bass.Bass("TRN2", target_bir_lowering=False)

    def din(name, shape):
        return nc.dram_tensor(name, list(shape), F32, kind="ExternalInput").ap()

    xT_d = din("xT", [nseq, 8, 128, S_LEN])
    cT_d = din("cT", [128, 8 * nseq])
    wada_d = din("w_ada", [2, D, 6 * D])
    bada_d = din("b_adaT", [128, 96])
    gvec_d = din("gvec", [128, 80])
    glat_d = din("glat", [128, 6])
    ggrp_d = din("g_group", [2, D])
    bfor_d = din("bfor", [2, 4])
    t5_d = din("t5T", [1, 128])
    win_d = din("w_in", [2, D, 1956])
    wuq_d = din("w_uq", [2, 256, 768])
    wukv_d = din("w_ukv", [2, 128, 1024])
    wout_d = din("w_out", [2, D, D])
    wgu_d = din("w_gate_up", [2, D, 2 * DFF])
    wdn_d = din("w_down", [2, DFF, D])
    cid_d = din("c_ident", [128, 128])
    cmask_d = din("c_mask", [128, 128])
    cD_d = din("c_D", [128, 256])
    crope_d = din("c_rope", [128, 2, S_LEN])
    outT_d = nc.dram_tensor("outT", [nseq, 8, 128, S_LEN], F32, kind="ExternalOutput").ap()
    dbg_d = {}
    if dbg:
        for nm, shp in (("d_h", [128, 8, S_LEN]), ("d_O", [128, 8, S_LEN]), ("d_xa", [128, 8, S_LEN]),
                        ("d_mod", [128, 96 * nseq])):
            dbg_d[nm] = nc.dram_tensor(nm, shp, F32, kind="ExternalOutput").ap()

    S = Sched(nc)
    A = nc.alloc_sbuf_tensor
    XT = A("XT", [128, 8, S_LEN], F32)
    HBt = A("HB", [128, 16384], BF16)
    HB = HBt[:, :].rearrange("p (a b) -> p a b", a=8)
    BIG = A("BIG", [128, 36864], BF16)

    def bview(off, n):
        return BIG[:, off:off + n]
    QT = [bview(j * 2048, 2048) for j in range(2)]
    KT = [bview(4096 + j * 2048, 2048) for j in range(2)]
    Vt = bview(8192, 4096).rearrange("p (t j d) -> p t j d", t=16, j=2)
    CQN = bview(12288, 4096).rearrange("p (a b) -> p a b", a=2)
    CKVN = bview(16384, 2048)
    KROT = bview(18432, 2048)
    SELT = BIG[32:48, 18432:20480]
    OTt = bview(20480, 16384).rearrange("p (k t) -> p k t", k=8)
    MF = BIG[:, 12288:16384].bitcast(F32)
    FF = BIG[:, 12288:18432].bitcast(F32)
    PRO = BIG[:, 32768:34880].bitcast(F32)
    WO = bview(12288, 8192).rearrange("p (a b) -> p a b", a=8)
    ACTT = bview(0, 22528).rearrange("p (a b) -> p a b", a=22)
    WGU = [bview(22528 + b * 4096, 4096).rearrange("p (k g n) -> p k g n", k=8, g=2) for b in range(2)]
    WD = [bview(30720 + b * 2816, 2816).rearrange("p (k n) -> p k n", k=22) for b in range(2)]
    BIGF = BIG[:, 0:32768].bitcast(F32)
    WADA = [BIGF[:, b * 8192:(b + 1) * 8192].rearrange("p (k n) -> p k n", k=8) for b in range(2)]

    ident_f = A("ident_f", [128, 128], F32)
    ident_b = A("ident_b", [128, 128], BF16)
    ones_b = A("ones_b", [128, 128], BF16)
    mask_b = A("mask_b", [128, 128], BF16)
    mask_f = PRO[:, 0:128]
    Dt = PRO[:, 128:384]
    Esel = A("Esel", [48, 16, 128], BF16)
    BIAS = A("BIAS", [128, 4, 2, 2, 128], BF16)
    tbl = PRO[:, 384:512].rearrange("p (h b) -> p h b", h=4)
    dlt = PRO[:, 512:640].rearrange("p (h b) -> p h b", h=4)
    gvec = A("gvec_s", [128, 5, 2, 8], F32)
    glat = A("glat_s", [128, 6], F32)
    bfor = A("bfor_s", [2, 4], F32)
    badaT = PRO[:, 640:736]
    cact = PRO[:, 736:736 + 8 * nseq].rearrange("p (k b) -> p k b", k=8)
    MODT = A("MODT", [128, 2, 48, nseq], F32)
    vec = A("vec", [128, 6, 8], F32)
    wst = A("wst", [128, 8, 416], BF16)
    wkrR = A("wkrR", [128, 8, 32], BF16)
    wuq = A("wuq_s", [128, 2, 192], BF16)
    wuqR = A("wuqR", [128, 2, 192], BF16)
    wukv = A("wukv_s", [128, 256], BF16)
    rope = A("rope", [128, 2, 512], F32)
    sq = [A(f"sq{i}", [128, 512], BF16) for i in range(2)]
    tmpf = [A(f"tmpf{i}", [128, 512], F32) for i in range(3)]
    lnv = A("lnv", [128, 512], F32)
    rstd = A("rstd", [128, 512], F32)
    PT = [A(f"PT{i}", [128, 512], BF16) for i in range(3)]
    Q32 = MF[:, 0:512]
    ksum = A("ksum", [128, 8], F32)
    Gt = MF[:, 512:768].rearrange("p (a n) -> p a n", a=32)
    cmpt = MF[:, 768:1280].rearrange("p (a n m) -> p a n m", a=8, n=8)
    rank = MF[:, 1280:1536].rearrange("p (a n) -> p a n", a=32)
    selneg = MF[:, 1536:1792].rearrange("p (a n) -> p a n", a=32)
    fe = FF[0:2, 0:512]
    fg = FF[0:2, 512:1024]
    Gc = [FF[0:2, 1024 + i * 512:1536 + i * 512] for i in range(2)]
    fR = FF[0:2, 2048:2560]
    fRall = BIG[0:2, 32768:36864].bitcast(F32)
    fhi = BIG[0:2, 17408:17920]
    fnhi = BIG[0:2, 17920:18432]
    bkT = A("bkT", [128, 16, 2], F32)
    negb = A("negb", [2, 4], F32)

    P = nc.alloc_psum_tensor
    psA = [P(f"psA{i}", [128, 512], F32) for i in range(3)]
    psPV = [P(f"psPV{i}", [128, 4, 128], F32) for i in range(2)]
    psM = [P(f"psM{i}", [128, 512], F32) for i in range(3)]
    rrA = [0]
    rrN = [0]
    psPVf = [t_[:, :, :].rearrange("p a b -> p (a b)") for t_ in psPV]
    rrM = [0]
    rrP = [0]
    rrS = [0]
    rrT = [0]

    def nextA():
        i = rrA[0] % 3
        rrA[0] += 1
        return psA[i], f"psA{i}"

    def nextM():
        i = rrM[0] % 3
        rrM[0] += 1
        return psM[i], f"psM{i}"

    def nextPT():
        i = rrP[0] % 3
        rrP[0] += 1
        return PT[i], f"PT{i}"

    def nextsq():
        i = rrS[0] % 2
        rrS[0] += 1
        return sq[i], f"sq{i}"

    def nexttmp():
        i = rrT[0] % 3
        rrT[0] += 1
        return tmpf[i], f"tmpf{i}"

    op = S.op

    def mm(out, lhsT, rhs, start, stop, r, w):
        return op("pe", r, w, lambda e: e.matmul(out, lhsT=lhsT, rhs=rhs, start=start, stop=stop,
                                                 skip_group_check=True))

    def cs(tc_):
        return slice(tc_ * 512, (tc_ + 1) * 512)

    S.dma("sp", ident_f[:], cid_d, [], ["ident_f"])
    S.dma("pool", ident_b[:], cid_d, [], ["ident_b"])
    S.dma("pool", mask_b[:], cmask_d, [], ["mask_b"])
    S.dma("sp", mask_f[:], cmask_d, [], ["mask_f"])
    S.dma("sp", Dt[:], cD_d, [], ["Dt"])
    S.dma("sp", tbl[:].rearrange("p h b -> p (h b)"), t5_d.partition_broadcast(128), [], ["tbl"])
    S.dma("sp", gvec[:].rearrange("p a l k -> p (a l k)"), gvec_d, [], ["gvec"])
    S.dma("sp", glat[:], glat_d, [], ["glat"])
    S.dma("sp", bfor[:], bfor_d, [], ["bfor"])
    S.dma("sp", badaT[:], bada_d, [], ["badaT"])
    S.dma("sp", PRO[:, 736:736 + 8 * nseq], cT_d, [], ["cact"])
    op("dve", [], ["ones_b"], lambda e: e.memset(ones_b[:], 1.0))
    op("dve", ["ident_b"], ["Esel"], lambda e: e.tensor_copy(
        out=Esel[32:48, :, :], in_=ident_b[32:48, 32:48].unsqueeze(2).to_broadcast([16, 16, 128])))
    op("dve", ["bfor"], ["negb"], lambda e: e.tensor_scalar(
        out=negb[:], in0=bfor[:], scalar1=-1.0, scalar2=None, op0=ALU.mult))
    op("act", ["cact"], ["cact"], lambda e: e.activation(out=cact[:], in_=cact[:], func=AF.Silu))

    lo_b = t5_lo_bounds()
    op("dve", ["tbl"], ["dlt"], lambda e: e.tensor_tensor(out=dlt[:, :, 1:32], in0=tbl[:, :, 1:32],
                                                          in1=tbl[:, :, 0:31], op=ALU.subtract))
    op("dve", ["tbl", "dlt"], ["dlt"], lambda e: e.tensor_tensor(out=dlt[:, :, 0:1], in0=tbl[:, :, 0:1],
                                                                 in1=tbl[:, :, 31:32], op=ALU.subtract))
    op("dve", ["dlt"], ["dlt"], lambda e: e.tensor_scalar(out=dlt[:, :, :], in0=dlt[:, :, :], scalar1=8.0, scalar2=None,
                                                          op0=ALU.mult))
    acc = tmpf[0]
    t2 = tmpf[1]
    for h in range(4):
        for ty in range(2):
            Dv = Dt[:, ty * 128:(ty + 1) * 128]
            op("dve", ["dlt", "Dt"], ["tmpf0"], lambda e, h=h: e.tensor_scalar(
                out=acc[:, 0:128], in0=Dv, scalar1=0.0, scalar2=dlt[:, h, 0:1], op0=ALU.mult, op1=ALU.add))
            for b in range(1, 32):
                if lo_b[b] > 255:
                    continue
                op("dve", ["dlt", "Dt"], ["tmpf1"], lambda e, h=h, b=b, Dv=Dv: e.tensor_scalar(
                    out=t2[:, 0:128], in0=Dv, scalar1=float(lo_b[b]) - 0.5, scalar2=dlt[:, h, b:b + 1],
                    op0=ALU.is_ge, op1=ALU.mult))
                op("dve", ["tmpf1", "tmpf0"], ["tmpf0"], lambda e: e.tensor_tensor(
                    out=acc[:, 0:128], in0=acc[:, 0:128], in1=t2[:, 0:128], op=ALU.add))
            op("dve", ["tmpf0"], ["BIAS"], lambda e, h=h, ty=ty: e.tensor_copy(out=BIAS[:, h, ty, 0, :], in_=acc[:, 0:128]))
            op("dve", ["tmpf0", "BIAS"], ["tmpf1"], lambda e, h=h, ty=ty: e.tensor_tensor(
                out=t2[:, 0:128], in0=acc[:, 0:128], in1=BIAS[:, h, ty, 0, :], op=ALU.subtract))
            op("dve", ["tmpf1"], ["BIAS"], lambda e, h=h, ty=ty: e.tensor_copy(out=BIAS[:, h, ty, 1, :], in_=t2[:, 0:128]))
            if ty == 0:
                op("dve", ["BIAS", "mask_f"], ["BIAS"], lambda e, h=h: e.tensor_tensor(
                    out=BIAS[:, h, 0, 0, :], in0=BIAS[:, h, 0, 0, :], in1=mask_f[:], op=ALU.add))

    for l in range(nlayers):
        for sl in range(6):
            b = (l * 6 + sl) % 2
            wk = f"wada{b}"
            S.dma("sp", WADA[b], wada_d[l].rearrange("(k p) n -> p k n", p=128)[:, :, sl * 1024:(sl + 1) * 1024],
                  [], [wk])
            ps, pk = nextM()
            for nn in range(8):
                for kc in range(8):
                    mm(ps[:, nn * nseq:(nn + 1) * nseq], WADA[b][:, kc, nn * 128:(nn + 1) * 128], cact[:, kc, :],
                       kc == 0, kc == 7, [wk, "cact"], [pk])
            op("dve", [pk, "badaT"], ["MODT"], lambda e, ps=ps, l=l, sl=sl: e.tensor_tensor(
                out=MODT[:, l, sl * 8:(sl + 1) * 8, :],
                in0=ps[:, 0:8 * nseq].rearrange("p (n b) -> p n b", n=8),
                in1=badaT[:, l * 48 + sl * 8: l * 48 + (sl + 1) * 8].unsqueeze(2).to_broadcast([128, 8, nseq]),
                op=ALU.add))
    if dbg:
        S.dma("sp", dbg_d["d_mod"], MODT[:].rearrange("p l n b -> p (l n b)"), ["MODT"], [])

    S.barrier()
    try:
        ck("pro")
    except _Stop:
        S.finish()
        return nc, S

    def rstd_from_ps(ps, pk, nfeat):
        op("act", [pk], ["lnv"], lambda e: e.activation(out=lnv[:], in_=ps[:], func=AF.Ln,
                                                         scale=1.0 / nfeat, bias=EPS))
        op("act", ["lnv"], ["rstd"], lambda e: e.activation(out=rstd[:], in_=lnv[:], func=AF.Exp, scale=-0.5))

    def norm_to_hb(tc_, si, bi):
        xk = f"x.{tc_}"
        ps, pk = nextM()
        for kc in range(8):
            s_, sk = nextsq()
            op("act", [xk], [sk], lambda e, s_=s_, kc=kc: e.activation(out=s_[:], in_=XT[:, kc, cs(tc_)], func=AF.Square))
            mm(ps[:], ones_b[:], s_[:], kc == 0, kc == 7, [sk, "ones_b"], [pk])
        rstd_from_ps(ps, pk, D)
        for kc in range(8):
            t_, tk = nexttmp()
            op("dve", [xk, "rstd"], [tk], lambda e, t_=t_, kc=kc: e.tensor_tensor(
                out=t_[:], in0=XT[:, kc, cs(tc_)], in1=rstd[:], op=ALU.mult))
            op("dve", [tk, "vec"], [f"h.{tc_}"], lambda e, t_=t_, kc=kc: e.tensor_scalar(
                out=HB[:, kc, cs(tc_)], in0=t_[:], scalar1=vec[:, si, kc:kc + 1], scalar2=vec[:, bi, kc:kc + 1],
                op0=ALU.mult, op1=ALU.add))

    def proj_fm(wcols, M, tc_, wkey, rd_extra=()):
        ps, pk = nextA()
        for kc in range(8):
            mm(ps[0:M, :], wst[:, kc, wcols], HB[:, kc, cs(tc_)], kc == 0, kc == 7,
               [wkey, f"h.{tc_}"] + list(rd_extra), [pk])
        return ps, pk

    def fox_cols(st):
        return [(0, 128 * st, 128), (128, 256 + 128 * st, 128), (256, 512 + 128 * st, 128), (384, 768 + 2 * st, 2)]

    def moba_cols(hp):
        return [(0, 772 + 128 * hp, 128), (128, 1028 + 128 * hp, 128), (256, 1284 + 128 * hp, 128)]

    def load_mla_w(l, ms):
        S.dma("pool", wuq[:], wuq_d[l].rearrange("(k p) n -> p k n", p=128)[:, :, 192 * ms:192 * ms + 192], [], ["wuq"])
        S.dma("pool", wukv[:], wukv_d[l][:, 256 * ms:256 * ms + 256], [], ["wukv"])

    def load_wst(l, colspecs):
        wv = win_d[l].rearrange("(k p) n -> p k n", p=128)
        for (dst0, src0, n) in colspecs:
            S.dma("pool", wst[:, :, dst0:dst0 + n], wv[:, :, src0:src0 + n], [], ["wst"])

    def v_proj(l_unused, wsl, src_kind):
        op("dve", [f"V.{c}" for c in range(4)], [f"V.{c}" for c in range(4)],
           lambda e: e.memset(Vt[:, :, :, 64:128], 1.0))
        for t in range(16):
            ps, pk = nextM()
            if src_kind == "h":
                for kc in range(8):
                    mm(ps[:, 0:128], HB[:, kc, t * 128:(t + 1) * 128], wst[:, kc, wsl], kc == 0, kc == 7,
                       ["wst", f"h.{t // 4}"], [pk])
                src = ps[:, 0:128].rearrange("p (j d) -> p j d", j=2)
            else:
                rhs = wukv[:, :].rearrange("p (j d) -> p j d", j=2)[:, :, 64:128]
                mm(ps[:, 0:128].rearrange("p (j d) -> p j d", j=2), CKVN[:, t * 128:(t + 1) * 128], rhs, True, True,
                   ["wukv", f"ckvn.{t // 4}"], [pk])
                src = ps[:, 0:128].rearrange("p (j d) -> p j d", j=2)
            op("act", [pk], [f"V.{t // 4}"], lambda e, t=t, src=src: e.activation(out=Vt[:, t, :, 0:64], in_=src, func=AF.Identity))

    def attention_stage(heads):
        steps = [(hd, c, kt) for hd in heads for c in range(4) for kt in range(4 * c + 4)]

        apool = [(psA[0], "psA0"), (psA[1], "psA1"), (psA[2], "psA2"), (psM[0], "psM0"), (psM[1], "psM1")]
        ppool = [(PT[0], "PT0"), (PT[1], "PT1"), (PT[2], "PT2"), (sq[0], "sq0"), (sq[1], "sq1")]
        rr_ = [0, 0]

        def emit_qk(step):
            (j, kind, hglob, kdim, scale, ocol), c, kt = step
            if kind == "moba":
                qsrc, ksrc = QT[0], KT[0]
                p0, p1 = 64 * j, 64 * j + 64
                qk_ = ["Q0", "K0"]
            else:
                qsrc, ksrc = QT[j], KT[j]
                p0, p1 = 0, kdim
                qk_ = [f"Q{j}", f"K{j}"]
            jq0 = max(4 * c, kt)
            q0 = jq0 * 128
            qend = (4 * c + 4) * 128
            W = qend - q0
            ps, pk = apool[rr_[0] % 5]
            rr_[0] += 1
            rd = [f"{qk_[0]}.{c}", f"{qk_[1]}.{kt // 4}"]
            lk = ksrc[p0:p1, kt * 128:(kt + 1) * 128]
            diag = kt >= 4 * c
            mm(ps[:, 0:W], lk, qsrc[p0:p1, q0:qend], True, False, rd, [pk])
            if kind == "moba":
                n = kt // 2
                mm(ps[:, 0:W], Esel[32:48, 8 * j + n, :], SELT[:, q0:qend], False, False,
                   ["Esel", f"selT.{c}"], [pk])
                if diag:
                    mm(ps[:, 0:128], ident_b[:], BIAS[:, hglob, 0, 0, :], False, False, ["ident_b", "BIAS"], [pk])
                    mm(ps[:, 0:128], ident_b[:], BIAS[:, hglob, 0, 1, :], False, False, ["ident_b", "BIAS"], [pk])
                if kt + 1 <= 4 * c + 3 and kt + 1 >= jq0:
                    o_ = (kt + 1) * 128 - q0
                    mm(ps[:, o_:o_ + 128], ident_b[:], BIAS[:, hglob, 1, 0, :], False, False, ["ident_b", "BIAS"], [pk])
                    mm(ps[:, o_:o_ + 128], ident_b[:], BIAS[:, hglob, 1, 1, :], False, True, ["ident_b", "BIAS"], [pk])
            else:
                if diag:
                    mm(ps[:, 0:128], ident_b[:], mask_b[:], False, True, ["ident_b", "mask_b"], [pk])
            pt, ptk = ppool[rr_[1] % 5]
            rr_[1] += 1
            if kind == "fox":
                op("act", [pk, "bkT"], [ptk], lambda e: e.activation(
                    out=pt[:, 0:W], in_=ps[:, 0:W], func=AF.Exp, scale=scale, bias=bkT[:, kt, j:j + 1]))
            else:
                op("act", [pk], [ptk], lambda e: e.activation(
                    out=pt[:, 0:W], in_=ps[:, 0:W], func=AF.Exp, scale=scale))
            return pt, ptk, jq0, q0

        def emit_pv(step, st_):
            (j, kind, hglob, kdim, scale, ocol), c, kt = step
            pt, ptk, jq0, q0 = st_
            acc_ = psPVf[c % 2]
            ak = f"psPV{c % 2}"
            W = (4 * c + 4) * 128 - q0
            off = q0 - 512 * c
            mm(acc_[:, off:off + W], Vt[:, kt, j, :], pt[:, 0:W], kt == 0, kt == 4 * c + 3,
               [ptk, f"V.{kt // 4}"], [ak])
            if kt == 4 * c + 3:
                i_ = rrN[0] % 2
                rrN[0] += 1
                rb, rbk = ((tmpf[0], "tmpf0"), (tmpf[1], "tmpf1"))[i_]
                op("dve", [ak], [rbk], lambda e: e.reciprocal(out=rb[0:64, :], in_=acc_[64:128, :]))
                pj = 64 * ((ocol % 128) // 64)
                op("dve", [ak, rbk], [f"OT.{c}"], lambda e: e.tensor_tensor(
                    out=OTt[pj:pj + 64, ocol // 128, cs(c)], in0=acc_[0:64, :], in1=rb[0:64, :], op=ALU.mult))

        LOOK = 4
        pend = []
        for si in range(len(steps) + LOOK):
            if si < len(steps):
                pend.append((steps[si], emit_qk(steps[si])))
            if si >= LOOK:
                stp, st_ = pend.pop(0)
                emit_pv(stp, st_)

    def layer(i, l):
        S.barrier()
        md = MODT[:, l, :, i]
        for (vi, mo, gi) in ((0, 8, 0), (3, 32, 2)):
            op("dve", ["MODT", "gvec"], ["vec"], lambda e, vi=vi, mo=mo, gi=gi: e.scalar_tensor_tensor(
                out=vec[:, vi, :], in0=md[:, mo:mo + 8], scalar=1.0, in1=gvec[:, gi, l, :], op0=ALU.add, op1=ALU.mult))
        for (vi, mo) in ((1, 0), (4, 24)):
            op("dve", ["MODT"], ["vec"], lambda e, vi=vi, mo=mo: e.tensor_copy(out=vec[:, vi, :], in_=md[:, mo:mo + 8]))
        for (vi, mo, gi) in ((2, 16, 1), (5, 40, 3)):
            op("dve", ["MODT", "gvec"], ["vec"], lambda e, vi=vi, mo=mo, gi=gi: e.tensor_tensor(
                out=vec[:, vi, :], in0=md[:, mo:mo + 8], in1=gvec[:, gi, l, :], op=ALU.mult))

        load_wst(l, fox_cols(0))
        for tc_ in range(4):
            norm_to_hb(tc_, 0, 1)
        if dbg and i == 0 and l == 0:
            for kc in range(8):
                t_, tk = nexttmp()
                for tc_ in range(4):
                    op("dve", [f"h.{tc_}"], [tk], lambda e, t_=t_, kc=kc, tc_=tc_: e.tensor_copy(out=t_[:], in_=HB[:, kc, cs(tc_)]))
                    S.dma("sp", dbg_d["d_h"][:, kc, cs(tc_)], t_[:], [tk], [])

        ck("norm")
        for st in range(2):
            deferred = []
            for j in range(2):
                op("dve", [f"Q{j}.{c}" for c in range(4)], [f"Q{j}.{c}" for c in range(4)],
                   lambda e, j=j: e.memset(QT[j][64:66, :], 1.0))
                op("dve", [f"K{j}.{c}" for c in range(4)], [f"K{j}.{c}" for c in range(4)],
                   lambda e, j=j: e.memset(KT[j][64:66, :], 1.0))
            for tc_ in range(4):
                ps, pk = proj_fm(slice(0, 128), 128, tc_, "wst")
                for j in range(2):
                    op("dve", [pk], [f"Q{j}.{tc_}"], lambda e, ps=ps, j=j: e.tensor_copy(
                        out=QT[j][0:64, cs(tc_)], in_=ps[64 * j:64 * j + 64, :]))
                ps, pk = proj_fm(slice(128, 256), 128, tc_, "wst")
                for j in range(2):
                    op("dve", [pk], [f"K{j}.{tc_}"], lambda e, ps=ps, j=j: e.tensor_copy(
                        out=KT[j][0:64, cs(tc_)], in_=ps[64 * j:64 * j + 64, :]))
                ps, pk = nextM()
                for kc in range(8):
                    mm(ps[0:2, :], wst[:, kc, 384:386], HB[:, kc, cs(tc_)], kc == 0, kc == 7, ["wst", f"h.{tc_}"], [pk])
                op("act", [pk, "negb"], ["fe"], lambda e, ps=ps: e.activation(
                    out=fe[:], in_=ps[0:2, :], func=AF.Exp, scale=-1.0, bias=negb[:, st * 2 + l: st * 2 + l + 1]))
                op("act", ["fe"], ["fe"], lambda e: e.activation(out=fe[:], in_=fe[:], func=AF.Ln, scale=1.0, bias=1.0))
                op("dve", ["fe"], ["fg"], lambda e: e.tensor_scalar(out=fg[:], in0=fe[:], scalar1=-4.0, scalar2=None, op0=ALU.mult))
                g_ = Gc[tc_ % 2]
                gk = f"Gc{tc_ % 2}"
                gp = Gc[(tc_ + 1) % 2]
                gpk = f"Gc{(tc_ + 1) % 2}"
                if tc_ == 0:
                    op("dve", ["fg"], [gk], lambda e, g_=g_: e.tensor_tensor_scan(
                        out=g_[:], data0=fg[:], data1=fg[:], initial=0.0, op0=ALU.add, op1=ALU.add))
                else:
                    op("dve", ["fg", gpk], [gk], lambda e, g_=g_, gp=gp: e.tensor_tensor_scan(
                        out=g_[:], data0=fg[:], data1=fg[:], initial=gp[:, 511:512], op0=ALU.add, op1=ALU.add))
                op("dve", [gk], ["fhi"], lambda e, g_=g_: e.tensor_copy(out=fhi[:], in_=g_[:]))
                op("dve", ["fhi"], ["fnhi"], lambda e: e.tensor_scalar(out=fnhi[:], in0=fhi[:], scalar1=-1.0, scalar2=None, op0=ALU.mult))
                op("dve", ["fhi", gk], ["fR"], lambda e, g_=g_: e.tensor_tensor(out=fR[:], in0=fhi[:], in1=g_[:], op=ALU.subtract))
                op("dve", ["fR"], ["fR"], lambda e: e.tensor_scalar(out=fR[:], in0=fR[:], scalar1=0.125, scalar2=None, op0=ALU.mult))
                for j in range(2):
                    S.dma("sp", QT[j][64:65, cs(tc_)], fhi[j:j + 1, :], ["fhi"], [f"Q{j}.{tc_}"])
                    S.dma("sp", KT[j][65:66, cs(tc_)], fnhi[j:j + 1, :], ["fnhi"], [f"K{j}.{tc_}"])
                op("dve", ["fR"], [f"fRall.{tc_}"], lambda e, tc_=tc_: e.tensor_copy(out=fRall[:, cs(tc_)], in_=fR[:]))
            ck("foxproj")
            v_proj(l, slice(256, 384), "h")
            load_wst(l, fox_cols(1) if st == 0 else moba_cols(0))
            for tt in range(16):
                ps2, pk2 = nextM()
                op("pe", [f"fRall.{tt // 4}", "ident_f"], [pk2], lambda e, ps2=ps2, tt=tt: e.transpose(
                    out=ps2[:, 0:2], in_=fRall[0:2, tt * 128:(tt + 1) * 128], identity=ident_f[0:2, 0:2]))
                op("dve", [pk2], ["bkT"], lambda e, ps2=ps2, tt=tt: e.tensor_copy(out=bkT[:, tt, :], in_=ps2[:, 0:2]))
            ck("foxv")
            attention_stage([(j, "fox", 2 * st + j, 66, 0.125, (st * 2 + j) * 64) for j in range(2)])
            ck("fox")

        S.barrier()
        ck("fox1")
        for hp in range(2):
            op("dve", [], ["Gt"], lambda e: e.memset(Gt[:], -1e30))
            op("dve", [], ["ksum"], lambda e: e.memset(ksum[:], 0.0))
            ck("m0a")
            for tc_ in range(4):
                ps, pk = proj_fm(slice(128, 256), 128, tc_, "wst")
                op("dve", [pk], [f"K0.{tc_}"], lambda e, ps=ps: e.tensor_copy(out=KT[0][:, cs(tc_)], in_=ps[:, :]))
                ck("m0b")
                op("dve", [pk], ["ksum"], lambda e, ps=ps: e.tensor_reduce(
                    out=ksum[:, 2 * tc_:2 * tc_ + 2], in_=ps[:, :].rearrange("p (b k) -> p b k", b=2), axis=AX.X, op=ALU.add))
                ck("m0c")
                ps, pk = proj_fm(slice(0, 128), 128, tc_, "wst")
                op("dve", [pk], [f"Q0.{tc_}"], lambda e, ps=ps: e.tensor_copy(out=QT[0][:, cs(tc_)], in_=ps[:, :]))
                op("dve", [pk], ["Q32"], lambda e, ps=ps: e.tensor_copy(out=Q32[:], in_=ps[:, :]))
                if tc_ == 0:
                    ck("m1")
                psg, pgk = nextM()
                for t in range(4):
                    for j in range(2):
                        mm(psg[:, (t * 2 + j) * 8:(t * 2 + j) * 8 + 8], Q32[64 * j:64 * j + 64, t * 128:(t + 1) * 128],
                           ksum[64 * j:64 * j + 64, :], True, True, ["Q32", "ksum"], [pgk])
                for half in range(2):
                    own = 2 * tc_ + half
                    if own == 0:
                        continue
                    op("dve", [pgk], ["Gt"], lambda e, psg=psg, half=half, own=own: e.tensor_copy(
                        out=Gt[:, (4 * tc_ + 2 * half) * 2:(4 * tc_ + 2 * half) * 2 + 4, 0:own],
                        in_=psg[:, half * 32:half * 32 + 32].rearrange("p (a n) -> p a n", a=4)[:, :, 0:own]))
            ck("m2")
            for q4 in range(4):
                gv = Gt[:, q4 * 8:(q4 + 1) * 8, :]
                op("dve", ["Gt"], ["cmpt"], lambda e, gv=gv: e.tensor_tensor(
                    out=cmpt[:], in0=gv.unsqueeze(2).to_broadcast([128, 8, 8, 8]),
                    in1=gv.unsqueeze(3).to_broadcast([128, 8, 8, 8]), op=ALU.is_gt))
                op("dve", ["cmpt"], ["rank"], lambda e, q4=q4: e.tensor_reduce(
                    out=rank[:, q4 * 8:(q4 + 1) * 8, :], in_=cmpt[:], axis=AX.X, op=ALU.add))
            op("dve", ["rank"], ["selneg"], lambda e: e.tensor_scalar(
                out=selneg[:], in0=rank[:], scalar1=3.0, scalar2=-NEG, op0=ALU.is_lt, op1=ALU.mult))
            op("dve", ["selneg"], ["selneg"], lambda e: e.tensor_scalar(
                out=selneg[:], in0=selneg[:], scalar1=NEG, scalar2=None, op0=ALU.add))
            for own in range(8):
                op("dve", ["selneg"], ["selneg"], lambda e, own=own: e.memset(selneg[:, own * 4:own * 4 + 4, own:own + 1], 0.0))
            ck("m3")
            v_proj(l, slice(256, 384), "h")
            load_wst(l, moba_cols(1) if hp == 0 else [(0, 1540, 416)])
            for t in range(16):
                ps2, pk2 = nextM()
                op("pe", ["selneg", "ident_f"], [pk2], lambda e, ps2=ps2, t=t: e.transpose(
                    out=ps2[0:16, 0:128], in_=selneg[:, 2 * t:2 * t + 2, :].rearrange("p a n -> p (a n)"), identity=ident_f[:]))
                op("dve", [pk2], [f"selT.{t // 4}"], lambda e, ps2=ps2, t=t: e.tensor_copy(
                    out=SELT[:, t * 128:(t + 1) * 128], in_=ps2[0:16, 0:128]))
            ck("mobaproj")
            attention_stage([(j, "moba", 2 * hp + j, 64, 0.125, 256 + (hp * 2 + j) * 64) for j in range(2)])
            ck("moba")

        S.barrier()
        ck("moba1")
        load_mla_w(l, 0)
        op("dve", ["wst"], ["wkrR"], lambda e: e.tensor_scalar(out=wkrR[:, :, 0:16], in0=wst[:, :, 400:416], scalar1=-1.0,
                                                               scalar2=None, op0=ALU.mult))
        op("dve", ["wst", "wkrR"], ["wkrR"], lambda e: e.tensor_copy(out=wkrR[:, :, 16:32], in_=wst[:, :, 384:400]))
        ck("p1")
        for tc_ in range(4):
            S.dma("sp", rope[:], crope_d[:, :, cs(tc_)], [], ["rope"])
            psn, pnk = nextM()
            for m in range(2):
                ps, pk = proj_fm(slice(128 * m, 128 * m + 128), 128, tc_, "wst")
                op("dve", [pk], [f"tmpf{m}"], lambda e, ps=ps, m=m: e.tensor_copy(out=tmpf[m][:], in_=ps[:, :]))
                s_, sk = nextsq()
                op("act", [f"tmpf{m}"], [sk], lambda e, m=m, s_=s_: e.activation(out=s_[:], in_=tmpf[m][:], func=AF.Square))
                mm(psn[:], ones_b[:], s_[:], m == 0, m == 1, [sk, "ones_b"], [pnk])
            rstd_from_ps(psn, pnk, 256)
            for m in range(2):
                op("dve", [f"tmpf{m}", "rstd", "glat"], [f"cqn.{tc_}"], lambda e, m=m: e.scalar_tensor_tensor(
                    out=CQN[:, m, cs(tc_)], in0=tmpf[m][:], scalar=glat[:, l * 2 + m:l * 2 + m + 1], in1=rstd[:],
                    op0=ALU.mult, op1=ALU.mult))
            ck("p2")
            psn, pnk = nextM()
            ps, pk = proj_fm(slice(256, 384), 128, tc_, "wst")
            op("dve", [pk], ["tmpf2"], lambda e, ps=ps: e.tensor_copy(out=tmpf[2][:], in_=ps[:, :]))
            s_, sk = nextsq()
            op("act", ["tmpf2"], [sk], lambda e, s_=s_: e.activation(out=s_[:], in_=tmpf[2][:], func=AF.Square))
            mm(psn[:], ones_b[:], s_[:], True, True, [sk, "ones_b"], [pnk])
            rstd_from_ps(psn, pnk, 128)
            op("dve", ["tmpf2", "rstd", "glat"], [f"ckvn.{tc_}"], lambda e: e.scalar_tensor_tensor(
                out=CKVN[:, cs(tc_)], in0=tmpf[2][:], scalar=glat[:, 4 + l:5 + l], in1=rstd[:],
                op0=ALU.mult, op1=ALU.mult))
            ck("p3")
            psa, pak = proj_fm(slice(384, 416), 32, tc_, "wst")
            psb, pbk = nextA()
            for kc in range(8):
                mm(psb[0:32, :], wkrR[:, kc, :], HB[:, kc, cs(tc_)], kc == 0, kc == 7, ["wkrR", f"h.{tc_}"], [pbk])
            t1, t1k = nexttmp()
            t2_, t2k = nexttmp()
            op("dve", [pak, "rope"], [t1k], lambda e, psa=psa, t1=t1: e.tensor_tensor(
                out=t1[0:32, :], in0=psa[0:32, :], in1=rope[0:32, 0, :], op=ALU.mult))
            op("dve", [pbk, "rope"], [t2k], lambda e, psb=psb, t2_=t2_: e.tensor_tensor(
                out=t2_[0:32, :], in0=psb[0:32, :], in1=rope[0:32, 1, :], op=ALU.mult))
            op("dve", [t1k, t2k], [f"krot.{tc_}"], lambda e, t1=t1, t2_=t2_: e.tensor_tensor(
                out=KROT[0:32, cs(tc_)], in0=t1[0:32, :], in1=t2_[0:32, :], op=ALU.add))

        ck("mlapre")
        sc_mla = 96 ** -0.5
        for ms in range(4):
            op("dve", [], ["wuqR"], lambda e: e.memset(wuqR[:], 0.0))
            wv = wuq[:].rearrange("p k (j d) -> p k j d", j=2)
            wr = wuqR[:].rearrange("p k (j d) -> p k j d", j=2)
            for k2 in range(2):
                op("dve", ["wuq", "wuqR"], ["wuqR"], lambda e, k2=k2: e.tensor_scalar(
                    out=wr[:, k2, :, 64:80], in0=wv[:, k2, :, 80:96], scalar1=-1.0, scalar2=None, op0=ALU.mult))
                op("dve", ["wuq", "wuqR"], ["wuqR"], lambda e, k2=k2: e.tensor_copy(out=wr[:, k2, :, 80:96], in_=wv[:, k2, :, 64:80]))
            for j in range(2):
                S.dma("sp", KT[j][64:96, :], KROT[0:32, :], [f"krot.{c}" for c in range(4)], [f"K{j}.{c}" for c in range(4)])
            for tc_ in range(4):
                S.dma("sp", rope[:], crope_d[:, :, cs(tc_)], [], ["rope"])
                for j in range(2):
                    psa, pak = nextA()
                    psb, pbk = nextA()
                    for k2 in range(2):
                        mm(psa[0:96, :], wuq[:, k2, 96 * j:96 * j + 96], CQN[:, k2, cs(tc_)], k2 == 0, k2 == 1,
                           ["wuq", f"cqn.{tc_}"], [pak])
                    for k2 in range(2):
                        mm(psb[0:96, :], wuqR[:, k2, 96 * j:96 * j + 96], CQN[:, k2, cs(tc_)], k2 == 0, k2 == 1,
                           ["wuqR", f"cqn.{tc_}"], [pbk])
                    op("dve", [pak], [f"Q{j}.{tc_}"], lambda e, psa=psa, j=j: e.tensor_copy(out=QT[j][0:64, cs(tc_)], in_=psa[0:64, :]))
                    t1, t1k = nexttmp()
                    t2_, t2k = nexttmp()
                    op("dve", [pak, "rope"], [t1k], lambda e, psa=psa, t1=t1: e.tensor_tensor(
                        out=t1[64:96, :], in0=psa[64:96, :], in1=rope[64:96, 0, :], op=ALU.mult))
                    op("dve", [pbk, "rope"], [t2k], lambda e, psb=psb, t2_=t2_: e.tensor_tensor(
                        out=t2_[64:96, :], in0=psb[64:96, :], in1=rope[64:96, 1, :], op=ALU.mult))
                    op("dve", [t1k, t2k], [f"Q{j}.{tc_}"], lambda e, t1=t1, t2_=t2_, j=j: e.tensor_tensor(
                        out=QT[j][64:96, cs(tc_)], in0=t1[64:96, :], in1=t2_[64:96, :], op=ALU.add))
                    psk, pkk = nextA()
                    mm(psk[0:64, :], wukv[:, 128 * j:128 * j + 64], CKVN[:, cs(tc_)], True, True, ["wukv", f"ckvn.{tc_}"], [pkk])
                    op("act", [pkk], [f"K{j}.{tc_}"], lambda e, psk=psk, j=j: e.activation(out=KT[j][0:64, cs(tc_)], in_=psk[0:64, :], func=AF.Identity))
            v_proj(l, None, "kv")
            if ms < 3:
                load_mla_w(l, ms + 1)
            else:
                S.dma("pool", WO, wout_d[l].rearrange("(k p) n -> p k n", p=128), [],
                      ["WO"] + [f"cqn.{c}" for c in range(4)] + [f"ckvn.{c}" for c in range(4)] + [f"krot.{c}" for c in range(4)])
            attention_stage([(j, "mla", 0, 96, sc_mla, 512 + (ms * 2 + j) * 64) for j in range(2)])

        if dbg and i == 0 and l == 0:
            for kc in range(8):
                for tc_ in range(4):
                    t_, tk = nexttmp()
                    op("dve", [f"OT.{tc_}"], [tk], lambda e, t_=t_, kc=kc, tc_=tc_: e.tensor_copy(out=t_[:], in_=OTt[:, kc, cs(tc_)]))
                    S.dma("sp", dbg_d["d_O"][:, kc, cs(tc_)], t_[:], [tk], [])
        S.barrier()
        OTCs = [HB[:, :, 1024:1536], bview(0, 4096).rearrange("p (a b) -> p a b", a=8)]
        rstdG = BIG[:, 4096:5120].bitcast(F32)
        lnvG = BIG[:, 5120:6144].bitcast(F32)
        groups = ((0, 2, 256), (2, 4, 256), (4, 8, 512))
        def gnorm(tc_):
            OTC = OTCs[tc_ % 2]
            otk = f"otc{tc_ % 2}"
            for gi_, (k0, k1, nf) in enumerate(groups):
                psg, pgk = (psM[0], "psM0") if (gi_ + tc_) % 2 == 0 else (psM[1], "psM1")
                for kc in range(k0, k1):
                    s_, sk = nextsq()
                    op("act", [f"OT.{tc_}"], [sk], lambda e, s_=s_, kc=kc: e.activation(
                        out=s_[:], in_=OTt[:, kc, cs(tc_)], func=AF.Square))
                    mm(psg[:], ones_b[:], s_[:], kc == k0, kc == k1 - 1, [sk, "ones_b"], [pgk])
                op("act", [pgk], ["lnvG"], lambda e, psg=psg, nf=nf: e.activation(
                    out=lnvG, in_=psg[:], func=AF.Ln, scale=1.0 / nf, bias=EPS))
                op("act", ["lnvG"], ["rstdG"], lambda e: e.activation(out=rstdG, in_=lnvG, func=AF.Exp, scale=-0.5))
                for kc in range(k0, k1):
                    op("dve", [f"OT.{tc_}", "rstdG", "gvec"], [otk], lambda e, kc=kc, OTC=OTC: e.scalar_tensor_tensor(
                        out=OTC[:, kc, :], in0=OTt[:, kc, cs(tc_)], scalar=gvec[:, 4, l, kc:kc + 1], in1=rstdG,
                        op0=ALU.mult, op1=ALU.mult))
        gnorm(0)
        for tc_ in range(4):
            OTC = OTCs[tc_ % 2]
            otk = f"otc{tc_ % 2}"
            resid_update(tc_, lambda n, OTC=OTC, otk=otk: (WO[:, :, n * 128:(n + 1) * 128], 8, lambda kc: OTC[:, kc, :], ["WO", otk]), 2,
                         lambda n: HB[:, n, 0:1024].bitcast(F32), ["y32a"],
                         mid=(lambda tc_=tc_: gnorm(tc_ + 1)) if tc_ < 3 else None)
        if dbg and i == 0 and l == 0:
            for kc in range(8):
                S.dma("sp", dbg_d["d_xa"][:, kc, :], XT[:, kc, :], [f"x.{c}" for c in range(4)], [])

        ck("outproj")
        S.barrier()
        fpool = [(psA[0], "psA0"), (psA[1], "psA1"), (psA[2], "psA2"), (psM[0], "psM0")]
        frr = [0]
        for hf in range(2):
            for tc_ in (2 * hf, 2 * hf + 1):
                norm_to_hb(tc_, 3, 4)
            for fb in range(11):
                b = fb % 2
                wk = f"wgu{b}"
                wv = wgu_d[l].rearrange("(k p) n -> p k n", p=128)
                S.dma("pool", WGU[b][:, :, 0, :], wv[:, :, fb * 256:(fb + 1) * 256], [], [wk])
                S.dma("pool", WGU[b][:, :, 1, :], wv[:, :, DFF + fb * 256:DFF + (fb + 1) * 256], [], [wk])
                for m in range(2):
                    for hh in range(2):
                        tc_ = 2 * hf + hh
                        psg, pgk = fpool[frr[0] % 4]
                        psu, puk = fpool[(frr[0] + 1) % 4]
                        frr[0] += 2
                        for kc in range(8):
                            mm(psg[:], WGU[b][:, kc, 0, m * 128:(m + 1) * 128], HB[:, kc, cs(tc_)], kc == 0, kc == 7,
                               [wk, f"h.{tc_}"], [pgk])
                        for kc in range(8):
                            mm(psu[:], WGU[b][:, kc, 1, m * 128:(m + 1) * 128], HB[:, kc, cs(tc_)], kc == 0, kc == 7,
                               [wk, f"h.{tc_}"], [puk])
                        pt, ptk = nextPT()
                        op("act", [pgk], [ptk], lambda e, pt=pt, psg=psg: e.activation(out=pt[:], in_=psg[:], func=AF.Silu))
                        op("dve", [ptk, puk], [f"act.{hh}"], lambda e, pt=pt, psu=psu, fb=fb, m=m, hh=hh: e.tensor_tensor(
                            out=ACTT[:, fb * 2 + m, hh * 512:(hh + 1) * 512], in0=pt[:], in1=psu[:], op=ALU.mult))
            resid_ffn2(l, hf)

    def resid_epilogue(tc_, y32f, ykf, psn, pnk, gi):
        rstd_from_ps(psn, pnk, D)
        for n in range(8):
            t_, tk = nexttmp()
            op("dve", ykf(n) + ["rstd"], [tk], lambda e, t_=t_, n=n: e.tensor_tensor(
                out=t_[:], in0=y32f(n), in1=rstd[:], op=ALU.mult))
            op("dve", [tk, "vec", f"x.{tc_}"], [f"x.{tc_}"], lambda e, t_=t_, n=n: e.scalar_tensor_tensor(
                out=XT[:, n, cs(tc_)], in0=t_[:], scalar=vec[:, gi, n:n + 1], in1=XT[:, n, cs(tc_)],
                op0=ALU.mult, op1=ALU.add))

    def resid_update(tc_, wfn, gi, y32f, ykeys, mid=None):
        psn = psM[2]
        pnk = "psM2"
        pend = []

        def flush():
            while pend:
                s0, sk0, n0 = pend.pop(0)
                mm(psn[:], ones_b[:], s0[:], n0 == 0, n0 == 7, [sk0, "ones_b"], [pnk])
        for n in range(8):
            lhs_all, nk, rhsf, rkeys = wfn(n)
            ps, pk = nextA()
            for kc in range(nk):
                mm(ps[:], lhs_all[:, kc, :], rhsf(kc), kc == 0, kc == nk - 1, rkeys, [pk])
            flush()
            op("dve", [pk], [f"y32a.{n}"], lambda e, ps=ps, n=n: e.tensor_copy(out=y32f(n), in_=ps[:]))
            s_, sk = nextsq()
            op("act", [f"y32a.{n}"], [sk], lambda e, n=n, s_=s_: e.activation(out=s_[:], in_=y32f(n), func=AF.Square))
            pend.append((s_, sk, n))
            if n == 2 and mid is not None:
                flush()
                mid()
        flush()
        resid_epilogue(tc_, y32f, lambda n: [f"y32a.{n}"], psn, pnk, gi)

    def resid_ffn2(l, hf):
        psn = [psM[1], psM[2]]
        pnk = ["psM1", "psM2"]
        regs = [hf, 1 - hf]
        coarse = [[f"h.{2 * r}", f"h.{2 * r + 1}"] for r in regs]

        def y32f(hh, n):
            return HB[:, n, regs[hh] * 1024:(regs[hh] + 1) * 1024].bitcast(F32)
        wv = wdn_d[l].rearrange("(k p) n -> p k n", p=128)
        pend = []

        def flush():
            while pend:
                s0, sk0, n0, h0 = pend.pop(0)
                mm(psn[h0][:], ones_b[:], s0[:], n0 == 0, n0 == 7, [sk0, "ones_b"], [pnk[h0]])
        for n in range(8):
            b = n % 2
            wk = f"wd{b}"
            S.dma("pool", WD[b], wv[:, :, n * 128:(n + 1) * 128], [], [wk])
            for hh in range(2):
                ps, pk = nextA()
                for k in range(22):
                    mm(ps[:], WD[b][:, k, :], ACTT[:, k, hh * 512:(hh + 1) * 512], k == 0, k == 21, [wk, f"act.{hh}"], [pk])
                flush()
                yk = [f"y.{hh}.{n}"] + (coarse[hh] if n == 0 else [])
                op("dve", [pk], yk, lambda e, ps=ps, n=n, hh=hh: e.tensor_copy(out=y32f(hh, n), in_=ps[:]))
                s_, sk = nextsq()
                op("act", [f"y.{hh}.{n}"], [sk], lambda e, n=n, hh=hh, s_=s_: e.activation(
                    out=s_[:], in_=y32f(hh, n), func=AF.Square))
                pend.append((s_, sk, n, hh))
        flush()
        for hh in range(2):
            resid_epilogue(2 * hf + hh, lambda n, hh=hh: y32f(hh, n), lambda n, hh=hh: [f"y.{hh}.{n}"] + coarse[hh],
                           psn[hh], pnk[hh], 5)

    psTb = psPV[1][:, :, :].rearrange("p a b -> p (a b)").bitcast(BF16).rearrange("p (k n) -> p k n", k=8)

    try:
        for i in range(nseq):
            for kc in range(8):
                S.dma("sp", XT[:, kc, :], xT_d[i, kc], [], [f"x.{c}" for c in range(4)])
            for l in range(nlayers):
                layer(i, l)
            for kc in range(8):
                S.dma("sp", outT_d[i, kc], XT[:, kc, :], [f"x.{c}" for c in range(4)], [])
    except _Stop:
        pass
    S.finish()
    return nc, S


def host_consts():
    ident = np.eye(128, dtype=np.float32)
    k = np.arange(128)[:, None]
    q = np.arange(128)[None, :]
    mask = np.where(k > q, NEG, 0.0).astype(np.float32)
    Dm = np.concatenate([(q - k), (q - k + 128)], axis=1).astype(np.float32)
    half = 16
    inv_freq = (1.0 / (10000.0 ** (np.arange(half, dtype=np.float32) / half))).astype(np.float32)
    ang = np.arange(S_LEN, dtype=np.float32)[None, :] * inv_freq[:, None]
    cos = np.cos(ang).astype(np.float32)
    sin = np.sin(ang).astype(np.float32)
    rope = np.zeros((128, 2, S_LEN), np.float32)
    for base in (0, 64):
        rope[base:base + 16, 0] = cos
        rope[base + 16:base + 32, 0] = cos
        rope[base:base + 16, 1] = sin
        rope[base + 16:base + 32, 1] = sin
    return ident, mask, Dm, rope


def make_in_maps(inputs, nseq, cores):
    f = lambda a: np.ascontiguousarray(np.asarray(a, dtype=np.float32))
    x = f(inputs["x"])
    c = f(inputs["c"])
    ident, mask, Dm, rope = host_consts()
    b_ada = f(inputs["b_ada"])
    badaT = np.ascontiguousarray(b_ada.reshape(2, 48, 128).transpose(2, 0, 1).reshape(128, 96))
    gv = np.stack([f(inputs[k]) for k in ("g_mix_pre", "g_mix_post", "g_ffn_pre", "g_ffn_post", "g_group")], 0)
    gvec = np.ascontiguousarray(gv.reshape(5, 2, 8, 128).transpose(3, 0, 1, 2).reshape(128, 80))
    gq = f(inputs["g_q_lat"]).reshape(2, 2, 128).transpose(2, 0, 1).reshape(128, 4)
    gkv = f(inputs["g_kv_lat"]).reshape(2, 128).T
    glat = np.ascontiguousarray(np.concatenate([gq, gkv], axis=1))
    bfo = f(inputs["b_forget"])
    bfor = np.zeros((2, 4), np.float32)
    for j in range(2):
        for st in range(2):
            for l in range(2):
                bfor[j, st * 2 + l] = bfo[l, 2 * st + j]
    t5T = np.ascontiguousarray(f(inputs["t5_table"]).T.reshape(1, 128))
    shared = {
        "w_ada": f(inputs["w_ada"]), "b_adaT": badaT, "gvec": gvec, "glat": glat, "g_group": f(inputs["g_group"]),
        "bfor": bfor, "t5T": t5T, "w_in": f(inputs["w_in"]), "w_uq": f(inputs["w_uq"]), "w_ukv": f(inputs["w_ukv"]),
        "w_out": f(inputs["w_out"]), "w_gate_up": f(inputs["w_gate_up"]), "w_down": f(inputs["w_down"]),
        "c_ident": ident, "c_mask": mask, "c_D": Dm, "c_rope": rope,
    }
    maps = []
    for ci in cores:
        xs = x[ci * nseq:(ci + 1) * nseq]
        xT = np.ascontiguousarray(xs.transpose(0, 2, 1).reshape(nseq, 8, 128, S_LEN))
        cs_ = c[ci * nseq:(ci + 1) * nseq]
        cT = np.ascontiguousarray(cs_.T.reshape(8, 128, nseq).transpose(1, 0, 2).reshape(128, 8 * nseq))
        m = dict(shared)
        m["xT"] = xT
        m["cT"] = cT
        maps.append(m)
    return maps


_CACHE = {}


def kernel(**inputs):
    if "nc" not in _CACHE:
        _CACHE["nc"] = build(NSEQ, 2, False)[0]
    nc = _CACHE["nc"]
    maps = make_in_maps(inputs, NSEQ, list(range(NCORES)))
    res = run_bass_kernel_spmd(nc, maps, core_ids=list(range(NCORES)))
    outs = []
    for r in res.results:
        oT = np.asarray(r["outT"]).reshape(NSEQ, D, S_LEN)
        outs.append(oT.transpose(0, 2, 1))
    return np.ascontiguousarray(np.concatenate(outs, axis=0).astype(np.float32))
```
